# Optimizing a Trainium2 kernel written in Bass

```python
import math
import jax, jax.numpy as jnp
from jax import lax
import numpy as np

D_MODEL = 1024
BATCH = 2
SEQ = 16384
DEPTH = 2

SGU_CHUNK = 128
SGU_GROUPS = 4
SGU_WIDTH = 512
SGU_GROUP_W = SGU_WIDTH // SGU_GROUPS
SSM_WIDTH = 512
SSM_GROUP = 16
SSM_GROUPS = SSM_WIDTH // SSM_GROUP
SSM_STATE = 64
DT_MIN = 0.001
DT_MAX = 0.1
ATT_HEADS = 8
HEAD_DIM = 64
ATT_WIDTH = ATT_HEADS * HEAD_DIM
MOBA_BLOCK = 256
MOBA_TOPK = 3
Q_CHUNK = 128
N_BRANCH = 3
IN_COLS = 2 * SGU_WIDTH + SSM_WIDTH + 3 * ATT_WIDTH + N_BRANCH * D_MODEL
D_FF = -(-8 * D_MODEL // (3 * 256)) * 256
DN_ALPHA = (2 * DEPTH) ** 0.25
DN_BETA = (8 * DEPTH) ** -0.25
LN_EPS = 1e-5
NEG_INF = -1e30

kernel_name = "gated_hybrid_gmlp_s5_moba_deepnorm"


def layer_norm(x, g, b):
    xf = x.astype(jnp.float32)
    mu = jnp.mean(xf, axis=-1, keepdims=True)
    var = jnp.mean(jnp.square(xf - mu), axis=-1, keepdims=True)
    return ((xf - mu) * lax.rsqrt(var + LN_EPS) * g.astype(jnp.float32) + b.astype(jnp.float32)).astype(x.dtype)


def spatial_gating(z, ln_g, ln_b, w_s, b_s):
    u, v = jnp.split(z, 2, axis=-1)
    v = layer_norm(v, ln_g, ln_b)
    bn, s, _ = v.shape
    v = v.reshape(bn, s // SGU_CHUNK, SGU_CHUNK, SGU_GROUPS, SGU_GROUP_W)
    w = jnp.tril(w_s)
    mixed = jnp.einsum('gts,bnsgc->bntgc', w, v) + b_s.T[:, :, None]
    return u * mixed.reshape(bn, s, SGU_WIDTH)


def s5_branch(u, lam_re, lam_im, log_dt, b_re, b_im, c_re, c_im, d, glu_w, glu_b):
    f32 = jnp.float32
    bn, s, _ = u.shape
    lam = lax.complex(lam_re.astype(f32), lam_im.astype(f32))
    dt = jnp.exp(log_dt.astype(f32))[:, None]
    lam_bar = jnp.exp(lam * dt)
    b_mat = lax.complex(b_re.astype(f32), b_im.astype(f32))
    b_bar = ((lam_bar - 1.0) / lam)[..., None] * b_mat
    uf = u.astype(f32)
    ug = uf.reshape(bn, s, SSM_GROUPS, SSM_GROUP).astype(jnp.complex64)
    bu = jnp.einsum('gph,bsgh->bsgp', b_bar, ug)
    a = jnp.broadcast_to(lam_bar, bu.shape)

    def combine(left, right):
        a_l, b_l = left
        a_r, b_r = right
        return a_r * a_l, a_r * b_l + b_r

    _, h = lax.associative_scan(combine, (a, bu), axis=1)
    c_mat = lax.complex(c_re.astype(f32), c_im.astype(f32))
    y = jnp.real(jnp.einsum('ghp,bsgp->bsgh', c_mat, h)).reshape(bn, s, SSM_WIDTH)
    y = jax.nn.gelu(y + d.astype(f32) * uf)
    y = y * jax.nn.sigmoid(y @ glu_w.astype(f32) + glu_b.astype(f32))
    return y.astype(u.dtype)


def moba_attention(q, k, v):
    f32 = jnp.float32
    bn, s = q.shape[0], q.shape[1]
    s_pad = -(-s // MOBA_BLOCK) * MOBA_BLOCK
    pad = ((0, 0), (0, s_pad - s), (0, 0), (0, 0))
    q = jnp.pad(q, pad).transpose(0, 2, 1, 3)
    k = jnp.pad(k, pad).transpose(0, 2, 1, 3)
    v = jnp.pad(v, pad).transpose(0, 2, 1, 3)
    nb = s_pad // MOBA_BLOCK
    n_sel = min(MOBA_TOPK, nb)
    kb = k.reshape(bn, ATT_HEADS, nb, MOBA_BLOCK, HEAD_DIM)
    vb = v.reshape(bn, ATT_HEADS, nb, MOBA_BLOCK, HEAD_DIM)
    k_mean = jnp.mean(kb.astype(f32), axis=3)
    pos = jnp.arange(s_pad, dtype=jnp.int32)
    q_blk = pos // MOBA_BLOCK
    gate = jnp.einsum('bhsd,bhnd->bhsn', q.astype(f32), k_mean)
    fully_past = jnp.arange(nb, dtype=jnp.int32)[None, :] < q_blk[:, None]
    gate = jnp.where(fully_past, gate, -jnp.inf)
    _, sel = lax.top_k(gate, n_sel)
    kb_flat = kb.reshape(bn * ATT_HEADS * nb, MOBA_BLOCK, HEAD_DIM)
    vb_flat = vb.reshape(bn * ATT_HEADS * nb, MOBA_BLOCK, HEAD_DIM)
    bh_off = (jnp.arange(bn * ATT_HEADS, dtype=jnp.int32) * nb).reshape(bn, ATT_HEADS, 1, 1)
    slopes = 2.0 ** (-8.0 * jnp.arange(1, ATT_HEADS + 1, dtype=f32) / ATT_HEADS)
    scale = HEAD_DIM ** -0.5
    n_chunks = s_pad // Q_CHUNK
    qc = q.reshape(bn, ATT_HEADS, n_chunks, Q_CHUNK, HEAD_DIM).transpose(2, 0, 1, 3, 4)
    selc = sel.reshape(bn, ATT_HEADS, n_chunks, Q_CHUNK, n_sel).transpose(2, 0, 1, 3, 4)
    chunk_ids = jnp.arange(n_chunks, dtype=jnp.int32)
    blk_ar = jnp.arange(MOBA_BLOCK, dtype=jnp.int32)

    def body(args):
        c, q_c, sel_c = args
        t = c * Q_CHUNK + jnp.arange(Q_CHUNK, dtype=jnp.int32)
        own = (c * Q_CHUNK) // MOBA_BLOCK
        kg = kb_flat[sel_c + bh_off]
        vg = vb_flat[sel_c + bh_off]
        s_g = jnp.einsum('bhqd,bhqkld->bhqkl', q_c, kg, preferred_element_type=f32) * scale
        key_pos_g = sel_c[..., None] * MOBA_BLOCK + blk_ar
        dist_g = jnp.abs(t[:, None, None] - key_pos_g).astype(f32)
        valid_g = jnp.arange(n_sel, dtype=jnp.int32)[None, :] < (t // MOBA_BLOCK)[:, None]
        s_g = jnp.where(valid_g[None, None, :, :, None], s_g - slopes[None, :, None, None, None] * dist_g, NEG_INF)
        k_own = lax.dynamic_index_in_dim(kb, own, axis=2, keepdims=False)
        v_own = lax.dynamic_index_in_dim(vb, own, axis=2, keepdims=False)
        s_o = jnp.einsum('bhqd,bhld->bhql', q_c, k_own, preferred_element_type=f32) * scale
        dist_o = t[:, None] - (own * MOBA_BLOCK + blk_ar)[None, :]
        s_o = jnp.where((dist_o >= 0)[None, None], s_o - slopes[None, :, None, None] * jnp.abs(dist_o).astype(f32), NEG_INF)
        scores = jnp.concatenate([s_g.reshape(bn, ATT_HEADS, Q_CHUNK, n_sel * MOBA_BLOCK), s_o], axis=-1)
        p = jax.nn.softmax(scores, axis=-1).astype(v.dtype)
        p_g = p[..., : n_sel * MOBA_BLOCK].reshape(bn, ATT_HEADS, Q_CHUNK, n_sel, MOBA_BLOCK)
        p_o = p[..., n_sel * MOBA_BLOCK:]
        out = (jnp.einsum('bhqkl,bhqkld->bhqd', p_g, vg, preferred_element_type=f32)
               + jnp.einsum('bhql,bhld->bhqd', p_o, v_own, preferred_element_type=f32))
        return out.astype(v.dtype)

    o = lax.map(body, (chunk_ids, qc, selc))
    o = o.transpose(1, 0, 3, 2, 4).reshape(bn, s_pad, ATT_WIDTH)
    return o[:, :s]


def hybrid_mixer(x, w_in, sgu_ln_g, sgu_ln_b, sgu_w, sgu_b, lam_re, lam_im, log_dt, b_re, b_im,
                 c_re, c_im, d, glu_w, glu_b, w_a, w_b, w_c, w_out):
    bn, s, _ = x.shape
    proj = x @ w_in
    splits = np.cumsum([2 * SGU_WIDTH, SSM_WIDTH, ATT_WIDTH, ATT_WIDTH, ATT_WIDTH]).tolist()
    z_sgu, z_ssm, q, k, v, gates = jnp.split(proj, splits, axis=-1)
    br_a = spatial_gating(jax.nn.gelu(z_sgu), sgu_ln_g, sgu_ln_b, sgu_w, sgu_b)
    br_b = s5_branch(z_ssm, lam_re, lam_im, log_dt, b_re, b_im, c_re, c_im, d, glu_w, glu_b)
    hd = (bn, s, ATT_HEADS, HEAD_DIM)
    br_c = moba_attention(q.reshape(hd), k.reshape(hd), v.reshape(hd))
    g = jax.nn.sigmoid(gates.astype(jnp.float32)).astype(x.dtype).reshape(bn, s, N_BRANCH, D_MODEL)
    merged = g[:, :, 0] * (br_a @ w_a) + g[:, :, 1] * (br_b @ w_b) + g[:, :, 2] * (br_c @ w_c)
    return merged @ w_out


def swiglu(x, w1, w3, w2):
    return (jax.nn.silu(x @ w1) * (x @ w3)) @ w2


def setup_inputs(seed: int = 0) -> dict:
    key = jax.random.key(seed)
    ks = jax.random.split(key, 32)
    L = DEPTH
    nrm = lambda k, shape, sc: jax.random.normal(k, shape, jnp.float32) * sc
    lam_im0 = jnp.pi * jnp.arange(SSM_STATE, dtype=jnp.float32)
    return {
        "x": nrm(ks[0], (BATCH, SEQ, D_MODEL), 1.0),
        "w_in": nrm(ks[1], (L, D_MODEL, IN_COLS), D_MODEL ** -0.5),
        "sgu_ln_g": 1.0 + nrm(ks[2], (L, SGU_WIDTH), 0.02),
        "sgu_ln_b": nrm(ks[3], (L, SGU_WIDTH), 0.02),
        "sgu_w": nrm(ks[4], (L, SGU_GROUPS, SGU_CHUNK, SGU_CHUNK), SGU_CHUNK ** -0.5),
        "sgu_b": 1.0 + nrm(ks[5], (L, SGU_GROUPS, SGU_CHUNK), 0.02),
        "ssm_lambda_re": -0.5 + nrm(ks[6], (L, SSM_GROUPS, SSM_STATE), 0.01),
        "ssm_lambda_im": lam_im0 + nrm(ks[7], (L, SSM_GROUPS, SSM_STATE), 0.01),
        "ssm_log_dt": jax.random.uniform(ks[8], (L, SSM_GROUPS), jnp.float32, math.log(DT_MIN), math.log(DT_MAX)),
        "ssm_b_re": nrm(ks[9], (L, SSM_GROUPS, SSM_STATE, SSM_GROUP), (2 * SSM_GROUP) ** -0.5),
        "ssm_b_im": nrm(ks[10], (L, SSM_GROUPS, SSM_STATE, SSM_GROUP), (2 * SSM_GROUP) ** -0.5),
        "ssm_c_re": nrm(ks[11], (L, SSM_GROUPS, SSM_GROUP, SSM_STATE), (2 * SSM_STATE) ** -0.5),
        "ssm_c_im": nrm(ks[12], (L, SSM_GROUPS, SSM_GROUP, SSM_STATE), (2 * SSM_STATE) ** -0.5),
        "ssm_d": nrm(ks[13], (L, SSM_WIDTH), 1.0),
        "glu_w": nrm(ks[14], (L, SSM_WIDTH, SSM_WIDTH), SSM_WIDTH ** -0.5),
        "glu_b": nrm(ks[15], (L, SSM_WIDTH), 0.02),
        "w_branch_a": nrm(ks[16], (L, SGU_WIDTH, D_MODEL), SGU_WIDTH ** -0.5),
        "w_branch_b": nrm(ks[17], (L, SSM_WIDTH, D_MODEL), SSM_WIDTH ** -0.5),
        "w_branch_c": nrm(ks[18], (L, ATT_WIDTH, D_MODEL), ATT_WIDTH ** -0.5),
        "w_out": nrm(ks[19], (L, D_MODEL, D_MODEL), DN_BETA * D_MODEL ** -0.5),
        "ln1_g": 1.0 + nrm(ks[20], (L, D_MODEL), 0.02),
        "ln1_b": nrm(ks[21], (L, D_MODEL), 0.02),
        "ffn_w1": nrm(ks[22], (L, D_MODEL, D_FF), D_MODEL ** -0.5),
        "ffn_w3": nrm(ks[23], (L, D_MODEL, D_FF), D_MODEL ** -0.5),
        "ffn_w2": nrm(ks[24], (L, D_FF, D_MODEL), DN_BETA * D_FF ** -0.5),
        "ln2_g": 1.0 + nrm(ks[25], (L, D_MODEL), 0.02),
        "ln2_b": nrm(ks[26], (L, D_MODEL), 0.02),
    }


def reference(x, w_in, sgu_ln_g, sgu_ln_b, sgu_w, sgu_b, ssm_lambda_re, ssm_lambda_im, ssm_log_dt,
              ssm_b_re, ssm_b_im, ssm_c_re, ssm_c_im, ssm_d, glu_w, glu_b, w_branch_a, w_branch_b,
              w_branch_c, w_out, ln1_g, ln1_b, ffn_w1, ffn_w3, ffn_w2, ln2_g, ln2_b):
    for l in range(DEPTH):
        mix = hybrid_mixer(x, w_in[l], sgu_ln_g[l], sgu_ln_b[l], sgu_w[l], sgu_b[l],
                           ssm_lambda_re[l], ssm_lambda_im[l], ssm_log_dt[l], ssm_b_re[l], ssm_b_im[l],
                           ssm_c_re[l], ssm_c_im[l], ssm_d[l], glu_w[l], glu_b[l],
                           w_branch_a[l], w_branch_b[l], w_branch_c[l], w_out[l])
        x = layer_norm(DN_ALPHA * x + mix, ln1_g[l], ln1_b[l])
        x = layer_norm(DN_ALPHA * x + swiglu(x, ffn_w1[l], ffn_w3[l], ffn_w2[l]), ln2_g[l], ln2_b[l])
    return x
```

```python
import numpy as np
import concourse.bass as bass
import concourse.mybir as mybir

F32 = mybir.dt.float32
BF16 = mybir.dt.bfloat16
I32 = mybir.dt.int32
AF = mybir.ActivationFunctionType
ALU = mybir.AluOpType
AX = mybir.AxisListType


class Buf:
    __slots__ = ("name", "lastw", "readers", "dsem", "dcount", "multi", "writers")

    def __init__(self, name, multi=False):
        self.name = name
        self.multi = multi
        self.writers = {}
        self.lastw = None
        self.readers = {}
        self.dsem = None
        self.dcount = 0


class Sched:
    ENG = ("pe", "dve", "act", "pool", "sp")

    def __init__(self, nc, stack):
        self.nc = nc
        self.stack = stack
        self.eng = {"pe": nc.tensor, "dve": nc.vector, "act": nc.scalar,
                    "pool": nc.gpsimd, "sp": nc.sync}
        self.ops = {e: [] for e in self.ENG}
        self.cnt = {e: 0 for e in self.ENG}
        self.sem = {}
        for e in ("pe", "dve", "act", "pool"):
            self.sem[e] = stack.enter_context(nc.semaphore("s_" + e))
        self.seen = {e: {} for e in self.ENG}
        self.nbuf = 0
        self.final_waits = []

    def buf(self, name=None):
        self.nbuf += 1
        return Buf(name or f"b{self.nbuf}")

    def bufs(self, n, name="b"):
        return [self.buf(f"{name}{i}") for i in range(n)]

    def _need(self, e, waits, key, val):
        if key is None:
            return
        if self.seen[e].get(key, 0) >= val:
            return
        waits[key] = max(waits.get(key, 0), val)

    def _deps(self, e, reads, writes, same_ok=False):
        waits = {}
        for b in reads:
            if b.multi:
                for k, v in b.writers.items():
                    self._need(e, waits, k, v)
            elif b.lastw is not None:
                k, v = b.lastw
                if not (same_ok and k == e):
                    self._need(e, waits, k, v)
        for b in writes:
            if b.multi:
                continue
            if b.lastw is not None:
                k, v = b.lastw
                if not (same_ok and k == e):
                    self._need(e, waits, k, v)
            for k, v in b.readers.items():
                if k == e:
                    continue
                self._need(e, waits, k, v)
        for k, v in waits.items():
            self.seen[e][k] = v
        return waits

    def op(self, e, fn, reads=(), writes=(), same_ok=False):
        waits = self._deps(e, reads, writes, same_ok)
        self.cnt[e] += 1
        n = self.cnt[e]
        self.ops[e].append((waits, fn, self.sem[e], 1))
        for b in reads:
            b.readers[e] = n
        for b in writes:
            b.lastw = (e, n)
            b.readers = {}

    def dma(self, q, fn, owner, reads=(), writes=()):
        if owner.dsem is None:
            self.nsem = getattr(self, "nsem", 0) + 1
            owner.dsem = self.stack.enter_context(self.nc.semaphore(f"d{self.nsem}_{owner.name}"))
            if not hasattr(self, "dma_owners"):
                self.dma_owners = []
            self.dma_owners.append(owner)
        waits = self._deps(q, reads, writes)
        key = owner
        waits.pop(key, None)
        owner.dcount += 16
        n = owner.dcount
        self.ops[q].append((waits, fn, owner.dsem, 16))
        for b in reads:
            b.readers[key] = n
        for b in writes:
            if b.multi:
                b.writers[key] = n
                continue
            b.lastw = (key, n)
            b.readers = {}

    def barrier(self):
        owners = getattr(self, "dma_owners", [])
        for e in self.ENG:
            waits = {}
            for k in ("pe", "dve", "act", "pool"):
                if k != e and self.cnt[k] > 0:
                    self._need(e, waits, k, self.cnt[k])
            for o in owners:
                if o.dcount > 0:
                    self._need(e, waits, o, o.dcount)
            for k, v in waits.items():
                self.seen[e][k] = v
            self.ops[e].append((waits, None, None, 0))

    def finish(self, e, bufs):
        waits = {}
        for b in bufs:
            for k, v in b.writers.items():
                self._need(e, waits, k, v)
            if b.lastw is not None:
                self._need(e, waits, *b.lastw)
            for k, v in b.readers.items():
                self._need(e, waits, k, v)
        self.ops[e].append((waits, None, None, 0))

    def _semof(self, key):
        if isinstance(key, Buf):
            return key.dsem
        return self.sem[key]

    def emit(self):
        nc = self.nc
        with nc.Block() as block:
            def mk(e):
                def body(engine):
                    for waits, fn, sem, inc in self.ops[e]:
                        for k, v in waits.items():
                            engine.wait_ge(self._semof(k), v)
                        if fn is not None:
                            fn(engine).then_inc(sem, inc)
                return body
            block.tensor(mk("pe"))
            block.vector(mk("dve"))
            block.scalar(mk("act"))
            block.gpsimd(mk("pool"))
            block.sync(mk("sp"))


class Ctx:
    def __init__(self, nc, stack):
        self.nc = nc
        self.st = stack
        self.S = Sched(nc, stack)
        self.n = 0
        self.rr = 0

    def sb(self, shape, dt, name=None):
        self.n += 1
        return self.st.enter_context(self.nc.sbuf_tensor(name or f"t{self.n}", list(shape), dt))

    def ps(self, shape, dt=F32, name=None):
        self.n += 1
        return self.st.enter_context(self.nc.psum_tensor(name or f"p{self.n}", list(shape), dt))

    def dram_in(self, name, shape, dt=F32):
        return self.nc.dram_tensor(name, list(shape), dt, kind="ExternalInput").ap()

    def dram_out(self, name, shape, dt=F32):
        return self.nc.dram_tensor(name, list(shape), dt, kind="ExternalOutput").ap()


import math
from contextlib import ExitStack

NT = 4096
NBLK = 8
TB = 512
T = 128
D = 1024
TWO_PI = 2 * math.pi


def sb(C, ph, shape, dt, name):
    C.n += 1
    return ph.enter_context(C.nc.sbuf_tensor(f"s{C.n}_{name}", list(shape), dt))


def range_reduce(C, eng, ang, tmpi, tmpf, Bang, Btmp):
    S = C.S
    S.op(eng, lambda e: e.tensor_scalar(out=tmpi, in0=ang, scalar1=1.0 / TWO_PI, scalar2=None, op0=ALU.mult), reads=[Bang], writes=[Btmp])
    S.op(eng, lambda e: e.tensor_copy(out=tmpf, in_=tmpi), reads=[Btmp], writes=[Btmp])
    S.op(eng, lambda e: e.scalar_tensor_tensor(out=ang, in0=tmpf, scalar=-TWO_PI, in1=ang, op0=ALU.mult, op1=ALU.add), reads=[Btmp, Bang], writes=[Bang])
    S.op(eng, lambda e: e.tensor_scalar(out=tmpf, in0=ang, scalar1=math.pi, scalar2=-TWO_PI, op0=ALU.is_gt, op1=ALU.mult), reads=[Bang], writes=[Btmp])
    S.op(eng, lambda e: e.tensor_tensor(out=ang, in0=ang, in1=tmpf, op=ALU.add), reads=[Bang, Btmp], writes=[Bang])
    S.op(eng, lambda e: e.tensor_scalar(out=tmpf, in0=ang, scalar1=-math.pi, scalar2=TWO_PI, op0=ALU.is_lt, op1=ALU.mult), reads=[Bang], writes=[Btmp])
    S.op(eng, lambda e: e.tensor_tensor(out=ang, in0=ang, in1=tmpf, op=ALU.add), reads=[Bang, Btmp], writes=[Bang])


def ssm_prep(C, ph, d):
    S = C.S
    nc = C.nc
    o = {}
    P16 = [128, 16]
    lamre = sb(C, ph, P16, F32, "lamre"); lamim = sb(C, ph, P16, F32, "lamim"); ldt = sb(C, ph, P16, F32, "ldt")
    Bp = S.buf("ssmp")
    S.dma("sp", lambda e: e.dma_start(out=lamre[:], in_=d["lamre"]), Bp, writes=[Bp])
    S.dma("sp", lambda e: e.dma_start(out=lamim[:], in_=d["lamim"]), Bp, writes=[Bp])
    S.dma("sp", lambda e: e.dma_start(out=ldt[:], in_=d["ldt"]), Bp, writes=[Bp])
    dt_ = sb(C, ph, P16, F32, "dt"); th = sb(C, ph, P16, F32, "th"); r = sb(C, ph, P16, F32, "r")
    ti = sb(C, ph, P16, I32, "ti"); tf = sb(C, ph, P16, F32, "tf"); a_ = sb(C, ph, P16, F32, "a_")
    Bs = S.buf("ssms"); Bth = S.buf("th"); Bt = S.buf("tmp")
    S.op("act", lambda e: e.activation(out=dt_[:], in_=ldt[:], func=AF.Exp), reads=[Bp], writes=[Bs])
    S.op("dve", lambda e: e.tensor_tensor(out=a_[:], in0=lamre[:], in1=dt_[:], op=ALU.mult), reads=[Bp, Bs], writes=[Bs])
    S.op("act", lambda e: e.activation(out=r[:], in_=a_[:], func=AF.Exp), reads=[Bs], writes=[Bs])
    S.op("dve", lambda e: e.tensor_tensor(out=th[:], in0=lamim[:], in1=dt_[:], op=ALU.mult), reads=[Bp, Bs], writes=[Bth])
    range_reduce(C, "dve", th[:], ti[:], tf[:], Bth, Bt)
    sn = sb(C, ph, P16, F32, "sn"); cs = sb(C, ph, P16, F32, "cs"); thc = sb(C, ph, P16, F32, "thc")
    Bc = S.buf("thc")
    S.op("act", lambda e: e.activation(out=sn[:], in_=th[:], func=AF.Sin), reads=[Bth], writes=[Bs])
    S.op("dve", lambda e: e.tensor_scalar(out=thc[:], in0=th[:], scalar1=math.pi / 2, scalar2=None, op0=ALU.add), reads=[Bth], writes=[Bc])
    range_reduce(C, "dve", thc[:], ti[:], tf[:], Bc, Bt)
    S.op("act", lambda e: e.activation(out=cs[:], in_=thc[:], func=AF.Sin), reads=[Bc], writes=[Bs])
    lbr = sb(C, ph, P16, F32, "lbr"); lbi = sb(C, ph, P16, F32, "lbi")
    S.op("dve", lambda e: e.tensor_tensor(out=lbr[:], in0=r[:], in1=cs[:], op=ALU.mult), reads=[Bs], writes=[Bs])
    S.op("dve", lambda e: e.tensor_tensor(out=lbi[:], in0=r[:], in1=sn[:], op=ALU.mult), reads=[Bs], writes=[Bs])
    nr = sb(C, ph, P16, F32, "nr"); den = sb(C, ph, P16, F32, "den"); t1 = sb(C, ph, P16, F32, "t1"); t2 = sb(C, ph, P16, F32, "t2")
    cr = sb(C, ph, P16, F32, "cr"); ci = sb(C, ph, P16, F32, "ci")
    S.op("dve", lambda e: e.tensor_scalar(out=nr[:], in0=lbr[:], scalar1=-1.0, scalar2=None, op0=ALU.add), reads=[Bs], writes=[Bs])
    S.op("dve", lambda e: e.tensor_tensor(out=t1[:], in0=lamre[:], in1=lamre[:], op=ALU.mult), reads=[Bp], writes=[Bs])
    S.op("dve", lambda e: e.tensor_tensor(out=t2[:], in0=lamim[:], in1=lamim[:], op=ALU.mult), reads=[Bp], writes=[Bs])
    S.op("dve", lambda e: e.tensor_tensor(out=den[:], in0=t1[:], in1=t2[:], op=ALU.add), reads=[Bs], writes=[Bs])
    S.op("dve", lambda e: e.reciprocal(out=den[:], in_=den[:]), reads=[Bs], writes=[Bs])
    S.op("dve", lambda e: e.tensor_tensor(out=t1[:], in0=nr[:], in1=lamre[:], op=ALU.mult), reads=[Bs, Bp], writes=[Bs])
    S.op("dve", lambda e: e.tensor_tensor(out=t2[:], in0=lbi[:], in1=lamim[:], op=ALU.mult), reads=[Bs, Bp], writes=[Bs])
    S.op("dve", lambda e: e.tensor_tensor(out=t1[:], in0=t1[:], in1=t2[:], op=ALU.add), reads=[Bs], writes=[Bs])
    S.op("dve", lambda e: e.tensor_tensor(out=cr[:], in0=t1[:], in1=den[:], op=ALU.mult), reads=[Bs], writes=[Bs])
    S.op("dve", lambda e: e.tensor_tensor(out=t1[:], in0=lbi[:], in1=lamre[:], op=ALU.mult), reads=[Bs, Bp], writes=[Bs])
    S.op("dve", lambda e: e.tensor_tensor(out=t2[:], in0=nr[:], in1=lamim[:], op=ALU.mult), reads=[Bs, Bp], writes=[Bs])
    S.op("dve", lambda e: e.tensor_tensor(out=t1[:], in0=t1[:], in1=t2[:], op=ALU.subtract), reads=[Bs], writes=[Bs])
    S.op("dve", lambda e: e.tensor_tensor(out=ci[:], in0=t1[:], in1=den[:], op=ALU.mult), reads=[Bs], writes=[Bs])
    o.update(th=th, r=r, lbr=lbr, lbi=lbi, cr=cr, ci=ci, Bs=Bs, Bth=Bth)
    return o


def trig_table(C, ph, th, Bth, name, n_st, taus, tmp_pool, Btaus):
    S = C.S
    n = taus.shape[1]
    CS = sb(C, ph, [128, n_st, 2, n], F32, name + "CS")
    SC = sb(C, ph, [128, n_st, 2, n], F32, name + "SC")
    B = S.buf(name)
    ang, angc, ti, tf, Ba, Bc, Bt = tmp_pool
    for st in range(n_st):
        S.op("dve", lambda e, st=st: e.tensor_scalar(out=ang[:, :n], in0=taus[:], scalar1=th[:, st:st + 1], scalar2=None, op0=ALU.mult), reads=[Bth, Btaus], writes=[Ba])
        range_reduce(C, "dve", ang[:, :n], ti[:, :n], tf[:, :n], Ba, Bt)
        S.op("act", lambda e, st=st: e.activation(out=CS[:, st, 1, :], in_=ang[:, :n], func=AF.Sin), reads=[Ba], writes=[B])
        S.op("dve", lambda e: e.tensor_scalar(out=angc[:, :n], in0=ang[:, :n], scalar1=math.pi / 2, scalar2=None, op0=ALU.add), reads=[Ba], writes=[Bc])
        range_reduce(C, "dve", angc[:, :n], ti[:, :n], tf[:, :n], Bc, Bt)
        S.op("act", lambda e, st=st: e.activation(out=CS[:, st, 0, :], in_=angc[:, :n], func=AF.Sin), reads=[Bc], writes=[B])
    S.op("pool", lambda e: e.tensor_copy(out=SC[:, :, 0, :], in_=CS[:, :, 1, :]), reads=[B], writes=[B])
    S.op("pool", lambda e: e.tensor_copy(out=SC[:, :, 1, :], in_=CS[:, :, 0, :]), reads=[B], writes=[B])
    return CS, SC, B


class Rot:
    def __init__(self, items):
        self.items = items
        self.i = 0

    def next(self):
        it = self.items[self.i % len(self.items)]
        self.i += 1
        return it


def ssm_tables(C, ph, d, consts, PS):
    S = C.S
    o = ssm_prep(C, ph, d)
    Bs, Bth = o["Bs"], o["Bth"]
    th = o["th"]
    ang = sb(C, ph, [128, 128], F32, "ang"); angc = sb(C, ph, [128, 128], F32, "angc")
    ti = sb(C, ph, [128, 128], I32, "tti"); tf = sb(C, ph, [128, 128], F32, "ttf")
    pool = (ang, angc, ti, tf, S.buf("ang"), S.buf("angc"), S.buf("ttmp"))
    CS, SC, Bcs = trig_table(C, ph, th, Bth, "rot", 16, consts["taus"], pool, consts["B"])
    CT, _, Bct = trig_table(C, ph, th, Bth, "rT", 16, consts["t128"], pool, consts["B"])
    o.update(CS=CS, SC=SC, Bcs=Bcs, CT=CT, Bct=Bct)
    R = sb(C, ph, [128, 16, 128], F32, "Rm"); Rp = sb(C, ph, [128, 16, 128], F32, "Rp")
    a_ = sb(C, ph, [128, 16], F32, "a2")
    BR = S.buf("R")
    S.op("act", lambda e: e.activation(out=a_[:], in_=o["r"][:], func=AF.Ln), reads=[Bs], writes=[BR])
    for st in range(16):
        S.op("pool", lambda e, st=st: e.tensor_scalar(out=R[:, st, :], in0=consts["ones"][:], scalar1=o["r"][:, st:st + 1], scalar2=None, op0=ALU.mult), reads=[Bs, consts["B"]], writes=[BR])
        S.op("act", lambda e, st=st: e.activation(out=Rp[:, st, :], in_=consts["taus1"][:], func=AF.Exp, scale=a_[:, st:st + 1]), reads=[BR, consts["B"]], writes=[BR])
    o.update(R=R, Rp=Rp, BR=BR)
    Bt_re = sb(C, ph, [128, 16, 128], F32, "Bt_re"); Bt_im = sb(C, ph, [128, 16, 128], F32, "Bt_im")
    Bld = S.buf("Btld")
    S.dma("sp", lambda e: e.dma_start(out=Bt_re[:], in_=d["Bt_re"]), Bld, writes=[Bld])
    S.dma("sp", lambda e: e.dma_start(out=Bt_im[:], in_=d["Bt_im"]), Bld, writes=[Bld])
    LB = sb(C, ph, [128, 16, 2, 128], BF16, "LB")
    BLB = S.buf("LB")
    Dm = Rot([(sb(C, ph, [128, 2, 128], F32, f"Dm{i}"), S.buf(f"Dm{i}")) for i in range(2)])
    tt = Rot([(sb(C, ph, [128, 4, 128], F32, f"tq{i}"), S.buf(f"tq{i}")) for i in range(2)])
    prot = Rot(PS[0:2])
    for st in range(16):
        Dt, BD = Dm.next()
        S.op("pool", lambda e, st=st, Dt=Dt: e.tensor_scalar(out=Dt[:, 0, :], in0=consts["ident"][:], scalar1=o["cr"][:, st:st + 1], scalar2=None, op0=ALU.mult), reads=[Bs, consts["B"]], writes=[BD])
        S.op("pool", lambda e, st=st, Dt=Dt: e.tensor_scalar(out=Dt[:, 1, :], in0=consts["ident"][:], scalar1=o["ci"][:, st:st + 1], scalar2=None, op0=ALU.mult), reads=[Bs, consts["B"]], writes=[BD])
        pt, Bpt = prot.next()
        S.op("pe", lambda e, Dt=Dt, pt=pt: e.matmul(pt[:, 0:256], lhsT=consts["ones"][:], rhs=Dt[:].rearrange("p a b -> p (a b)"), start=True, stop=True), reads=[BD, consts["B"]], writes=[Bpt])
        tq, Bq = tt.next()
        S.op("dve", lambda e, st=st, pt=pt, tq=tq: e.tensor_tensor(out=tq[:, 0, :], in0=pt[:, 0:128], in1=Bt_re[:, st, :], op=ALU.mult), reads=[Bpt, Bld], writes=[Bq])
        S.op("dve", lambda e, st=st, pt=pt, tq=tq: e.tensor_tensor(out=tq[:, 1, :], in0=pt[:, 128:256], in1=Bt_im[:, st, :], op=ALU.mult), reads=[Bpt, Bld], writes=[Bq])
        S.op("dve", lambda e, st=st, pt=pt, tq=tq: e.tensor_tensor(out=tq[:, 2, :], in0=pt[:, 0:128], in1=Bt_im[:, st, :], op=ALU.mult), reads=[Bpt, Bld], writes=[Bq])
        S.op("dve", lambda e, st=st, pt=pt, tq=tq: e.tensor_tensor(out=tq[:, 3, :], in0=pt[:, 128:256], in1=Bt_re[:, st, :], op=ALU.mult), reads=[Bpt, Bld], writes=[Bq])
        S.op("pool", lambda e, st=st, tq=tq: e.tensor_tensor(out=LB[:, st, 0, :], in0=tq[:, 0, :], in1=tq[:, 1, :], op=ALU.subtract), reads=[Bq], writes=[BLB])
        S.op("pool", lambda e, st=st, tq=tq: e.tensor_tensor(out=LB[:, st, 1, :], in0=tq[:, 2, :], in1=tq[:, 3, :], op=ALU.add), reads=[Bq], writes=[BLB])
    Ct_re = Bt_re; Ct_im = Bt_im
    LC = sb(C, ph, [128, 16, 2, 128], BF16, "LC")
    BLC = S.buf("LC")
    S.dma("sp", lambda e: e.dma_start(out=Ct_re[:], in_=d["Ct_re"]), Bld, reads=[BLB], writes=[Bld])
    S.dma("sp", lambda e: e.dma_start(out=Ct_im[:], in_=d["Ct_im"]), Bld, reads=[BLB], writes=[Bld])
    S.op("pool", lambda e: e.tensor_copy(out=LC[:, :, 0, :], in_=Ct_re[:]), reads=[Bld], writes=[BLC])
    S.op("pool", lambda e: e.tensor_scalar(out=LC[:, :, 1, :], in0=Ct_im[:], scalar1=-1.0, scalar2=None, op0=ALU.mult), reads=[Bld], writes=[BLC])
    o.update(LB=LB, BLB=BLB, LC=LC, BLC=BLC)
    return o


def ssm_state_bufs(C, ph):
    S = C.S
    o = {}
    o["GG"] = sb(C, ph, [128, 16, 2, 128], F32, "GG")
    o["BGG"] = [S.buf(f"GG{st}") for st in range(16)]
    o["H"] = sb(C, ph, [128, 16, 2, 128], BF16, "Hb")
    o["BH"] = [S.buf(f"H{st}") for st in range(16)]
    o["init"] = sb(C, ph, [128, 16, 2], F32, "ginit")
    o["Binit"] = S.buf("ginit")
    o["itmp"] = sb(C, ph, [128, 4, 16], F32, "itmp")
    o["Bitmp"] = S.buf("itmp")
    o["rt"] = Rot([(sb(C, ph, [128, 2, 2, 128], F32, f"rt{i}"), S.buf(f"rt{i}")) for i in range(3)])
    o["bp"] = Rot([(sb(C, ph, [128, 2, 128], F32, f"bp{i}"), S.buf(f"bp{i}")) for i in range(3)])
    o["pt"] = Rot([(sb(C, ph, [128, 2, 2, 128], F32, f"pq{i}"), S.buf(f"pq{i}")) for i in range(3)])
    return o


def ssm_rot_out_and_C(C, tb, sbf, st_list, PSC, BPSC):
    S = C.S
    GG, H = sbf["GG"], sbf["H"]
    CS, SC, Bcs = tb["CS"], tb["SC"], tb["Bcs"]
    for st in st_list:
        p12, Bp = sbf["pt"].next()
        S.op("pool", lambda e, st=st, p12=p12: e.tensor_tensor(out=p12[:, 0], in0=CS[:, st], in1=GG[:, st], op=ALU.mult), reads=[Bcs, sbf["BGG"][st]], writes=[Bp])
        S.op("pool", lambda e, st=st, p12=p12: e.tensor_tensor(out=p12[:, 1], in0=SC[:, st], in1=GG[:, st], op=ALU.mult), reads=[Bcs, sbf["BGG"][st]], writes=[Bp])
        S.op("pool", lambda e, st=st, p12=p12: e.tensor_tensor(out=H[:, st, 0, :], in0=p12[:, 0, 0, :], in1=p12[:, 0, 1, :], op=ALU.subtract), reads=[Bp], writes=[sbf["BH"][st]])
        S.op("pool", lambda e, st=st, p12=p12: e.tensor_tensor(out=H[:, st, 1, :], in0=p12[:, 1, 0, :], in1=p12[:, 1, 1, :], op=ALU.add), reads=[Bp], writes=[sbf["BH"][st]])
    LC, BLC = tb["LC"], tb["BLC"]
    for ct in range(4):
        n = 0
        for st in range(4 * ct, 4 * ct + 4):
            for ri in range(2):
                S.op("pe", lambda e, ct=ct, st=st, ri=ri, n=n: e.matmul(PSC[:, ct * 128:(ct + 1) * 128], lhsT=LC[:, st, ri, :], rhs=H[:, st, ri, :], start=(n == 0), stop=(n == 7)),
                     reads=[BLC, sbf["BH"][st]], writes=[BPSC], same_ok=True)
                n += 1


def ssm_next_init(C, tb, sbf, table, Btable, col, out, Bout):
    S = C.S
    GG = sbf["GG"]
    it, Bit = sbf["itmp"], sbf["Bitmp"]
    c = table[:, :, 0, col]; s = table[:, :, 1, col]
    glr = GG[:, :, 0, 127]; gli = GG[:, :, 1, 127]
    allg = sbf["BGG"]
    S.op("dve", lambda e: e.tensor_tensor(out=it[:, 0, :], in0=c, in1=glr, op=ALU.mult), reads=[Btable] + allg, writes=[Bit])
    S.op("dve", lambda e: e.tensor_tensor(out=it[:, 1, :], in0=s, in1=gli, op=ALU.mult), reads=[Btable] + allg, writes=[Bit])
    S.op("dve", lambda e: e.tensor_tensor(out=it[:, 2, :], in0=s, in1=glr, op=ALU.mult), reads=[Btable] + allg, writes=[Bit])
    S.op("dve", lambda e: e.tensor_tensor(out=it[:, 3, :], in0=c, in1=gli, op=ALU.mult), reads=[Btable] + allg, writes=[Bit])
    S.op("dve", lambda e: e.tensor_tensor(out=out[:, :, 0], in0=it[:, 0, :], in1=it[:, 1, :], op=ALU.subtract), reads=[Bit], writes=[Bout])
    S.op("dve", lambda e: e.tensor_tensor(out=out[:, :, 1], in0=it[:, 2, :], in1=it[:, 3, :], op=ALU.add), reads=[Bit], writes=[Bout])


def ssm_local_tile(C, tb, sbf, u_bf, Bu, tok0, first, PSB, PSC, BPSC):
    S = C.S
    LB, BLB = tb["LB"], tb["BLB"]
    CS, SC, Bcs = tb["CS"], tb["SC"], tb["Bcs"]
    GG = sbf["GG"]
    if first:
        S.op("pool", lambda e: e.memset(sbf["init"][:], 0.0), writes=[sbf["Binit"]])
    for st in range(16):
        ct = st // 4
        pb, Bpb = PSB.next()
        for ri in range(2):
            S.op("pe", lambda e, st=st, ri=ri, pb=pb, ct=ct: e.matmul(pb[:, ri * 128:(ri + 1) * 128], lhsT=LB[:, st, ri, :], rhs=u_bf[:, ct, tok0:tok0 + 128], start=True, stop=True),
                 reads=[BLB, Bu], writes=[Bpb], same_ok=True)
        bu = pb[:, 0:256].rearrange("p (a b) -> p a b", a=2)
        rt, Brt = sbf["rt"].next()
        S.op("dve", lambda e, st=st, rt=rt, bu=bu: e.tensor_tensor(out=rt[:, 0], in0=bu, in1=CS[:, st], op=ALU.mult), reads=[Bpb, Bcs], writes=[Brt])
        S.op("dve", lambda e, st=st, rt=rt, bu=bu: e.tensor_tensor(out=rt[:, 1], in0=bu, in1=SC[:, st], op=ALU.mult), reads=[Bpb, Bcs], writes=[Brt])
        bp, Bbp = sbf["bp"].next()
        S.op("dve", lambda e, rt=rt, bp=bp: e.tensor_tensor(out=bp[:, 0, :], in0=rt[:, 0, 0, :], in1=rt[:, 0, 1, :], op=ALU.add), reads=[Brt], writes=[Bbp])
        S.op("dve", lambda e, rt=rt, bp=bp: e.tensor_tensor(out=bp[:, 1, :], in0=rt[:, 1, 1, :], in1=rt[:, 1, 0, :], op=ALU.subtract), reads=[Brt], writes=[Bbp])
        for ri in range(2):
            S.op("dve", lambda e, st=st, ri=ri, bp=bp: e.tensor_tensor_scan(out=GG[:, st, ri, :], data0=tb["R"][:, st, :], data1=bp[:, ri, :], initial=sbf["init"][:, st, ri:ri + 1], op0=ALU.mult, op1=ALU.add),
                 reads=[tb["BR"], Bbp, sbf["Binit"]], writes=[sbf["BGG"][st]])
    ssm_rot_out_and_C(C, tb, sbf, range(16), PSC, BPSC)


def ssm_corr_tile(C, tb, sbf, PSC, BPSC):
    S = C.S
    GG = sbf["GG"]
    for st in range(16):
        for ri in range(2):
            S.op("dve", lambda e, st=st, ri=ri: e.tensor_scalar(out=GG[:, st, ri, :], in0=tb["Rp"][:, st, :], scalar1=sbf["init"][:, st, ri:ri + 1], scalar2=None, op0=ALU.mult),
                 reads=[tb["BR"], sbf["Binit"]], writes=[sbf["BGG"][st]])
    ssm_rot_out_and_C(C, tb, sbf, range(16), PSC, BPSC)


def load_consts(C, ph, d):
    S = C.S
    o = {}
    B = S.buf("consts")
    for nm, shape in (("ident", [128, 128]), ("ones", [128, 128]), ("taus", [128, 128]), ("taus1", [128, 128]), ("t128", [128, 1]), ("trimask", [128, 128])):
        t = sb(C, ph, shape, F32, "c_" + nm)
        S.dma("sp", lambda e, t=t, nm=nm: e.dma_start(out=t[:], in_=d[nm]), B, writes=[B])
        o[nm] = t
    o["B"] = B
    identb = sb(C, ph, [128, 128], BF16, "c_identb")
    S.op("dve", lambda e: e.tensor_copy(out=identb[:], in_=o["ident"][:]), reads=[B], writes=[B])
    o["identb"] = identb
    return o


def load_w_bf16(C, ph, w_dram, col0, ncols, name, stage, kchunks=8):
    S = C.S
    wb = sb(C, ph, [128, kchunks, ncols], BF16, name)
    Bw = S.buf(name)
    engs = ("pool", "act")
    n = 0
    for kc in range(kchunks):
        for c0 in range(0, ncols, 2048):
            cw = min(2048, ncols - c0)
            stg, Bst = stage.next()
            S.dma("sp", lambda e, kc=kc, c0=c0, cw=cw, stg=stg: e.dma_start(out=stg[:, :cw], in_=w_dram[kc * 128:(kc + 1) * 128, col0 + c0:col0 + c0 + cw]), Bst, writes=[Bst])
            eng = engs[n % 2]
            n += 1
            if eng == "act":
                S.op("act", lambda e, kc=kc, c0=c0, cw=cw, stg=stg: e.copy(out=wb[:, kc, c0:c0 + cw], in_=stg[:, :cw]), reads=[Bst], writes=[Bw])
            else:
                S.op("pool", lambda e, kc=kc, c0=c0, cw=cw, stg=stg: e.tensor_copy(out=wb[:, kc, c0:c0 + cw], in_=stg[:, :cw]), reads=[Bst], writes=[Bw])
    return wb, Bw


def proj_fm(C, PSr, xb, Bx, wb, Bw, col, ntok=512, kchunks=8, tok0=0):
    S = C.S
    pt, Bp = PSr.next()
    for kc in range(kchunks):
        S.op("pe", lambda e, kc=kc, pt=pt: e.matmul(pt[:, 0:ntok], lhsT=wb[:, kc, col:col + 128], rhs=xb[:, kc, tok0:tok0 + ntok], start=(kc == 0), stop=(kc == kchunks - 1)),
             reads=[Bx, Bw], writes=[Bp], same_ok=True)
    return pt, Bp


def proj_tm(C, PSr, xb, Bx, wb, Bw, col, tok0, ncols=512, kchunks=8):
    S = C.S
    pt, Bp = PSr.next()
    for kc in range(kchunks):
        S.op("pe", lambda e, kc=kc, pt=pt: e.matmul(pt[:, 0:ncols], lhsT=xb[:, kc, tok0:tok0 + 128], rhs=wb[:, kc, col:col + ncols], start=(kc == 0), stop=(kc == kchunks - 1)),
             reads=[Bx, Bw], writes=[Bp], same_ok=True)
    return pt, Bp


def layer_norm_tm(C, eng_pool, x, Bx, out, Bout, g_rep, b_rep, Bgb, stat, Bstat, width):
    S = C.S
    nch = (width + 511) // 512
    st6, mv, rstd = stat
    for c in range(nch):
        S.op("dve", lambda e, c=c: e.bn_stats(out=st6[:, c, :], in_=x[:, c * 512:min(width, (c + 1) * 512)]), reads=[Bx], writes=[Bstat])
    S.op("dve", lambda e: e.bn_aggr(out=mv[:], in_=st6[:, 0:nch, :]), reads=[Bstat], writes=[Bstat])
    S.op("act", lambda e: e.activation(out=rstd[:], in_=mv[:, 1:2], func=AF.Sqrt, bias=C.eps[:, 0:1], scale=1.0), reads=[Bstat], writes=[Bstat])
    S.op("dve", lambda e: e.reciprocal(out=rstd[:], in_=rstd[:]), reads=[Bstat], writes=[Bstat])
    S.op("dve", lambda e: e.tensor_scalar(out=x, in0=x, scalar1=mv[:, 0:1], scalar2=rstd[:, 0:1], op0=ALU.subtract, op1=ALU.mult), reads=[Bx, Bstat], writes=[Bx])
    S.op(eng_pool, lambda e: e.tensor_tensor(out=x, in0=x, in1=g_rep, op=ALU.mult), reads=[Bx, Bgb], writes=[Bx])
    S.op(eng_pool, lambda e: e.tensor_tensor(out=out, in0=x, in1=b_rep, op=ALU.add), reads=[Bx, Bgb], writes=[Bout])


def load_xblock(C, d, i, xs, Bxs, xb, Bxb):
    S = C.S
    for h in range(2):
        S.dma("sp", lambda e, i=i, h=h: e.dma_start(out=xs[:], in_=d["xT"][h * 512:(h + 1) * 512, i * 512:(i + 1) * 512].rearrange("(k p) m -> p k m", p=128)), Bxs, reads=[C.BoutT_dram], writes=[Bxs])
        S.op("pool", lambda e, h=h: e.tensor_copy(out=xb[:, h * 4:(h + 1) * 4, :], in_=xs[:]), reads=[Bxs], writes=[Bxb])


def phase_A_sgu(C, d, consts, PS):
    S = C.S
    with ExitStack() as ph:
        stage = Rot([(sb(C, ph, [128, 2048], F32, f"wst{i}"), S.buf(f"wst{i}")) for i in range(2)])
        wb, Bw = load_w_bf16(C, ph, d["w_in"], 0, 1024, "wA1", stage)
        Bsm = S.buf("smallA1")
        g_rep = sb(C, ph, [128, 512], F32, "sgug"); b_rep = sb(C, ph, [128, 512], F32, "sgub")
        wsT = sb(C, ph, [128, 4, 128], F32, "wsT"); bs_rep = sb(C, ph, [128, 4, 128], F32, "bsrep")
        for t, nm in ((g_rep, "sgu_g_rep"), (b_rep, "sgu_b_rep"), (wsT, "sgu_wT"), (bs_rep, "sgu_bs_rep")):
            S.dma("sp", lambda e, t=t, nm=nm: e.dma_start(out=t[:], in_=d[nm]), Bsm, writes=[Bsm])
        wsb = sb(C, ph, [128, 4, 128], BF16, "wsb")
        Bwsb = S.buf("wsb")
        for g in range(4):
            S.op("dve", lambda e, g=g: e.tensor_tensor(out=wsb[:, g, :], in0=wsT[:, g, :], in1=consts["trimask"][:], op=ALU.mult), reads=[Bsm, consts["B"]], writes=[Bwsb])
        xs = sb(C, ph, [128, 4, 512], F32, "xs"); Bxs = S.buf("xs")
        xbr = Rot([(sb(C, ph, [128, 8, 512], BF16, f"xb{i}"), S.buf(f"xb{i}")) for i in range(2)])
        uT = sb(C, ph, [128, 4, 512], BF16, "uT"); BuT = S.buf("uT")
        vg = Rot([(sb(C, ph, [128, 512], F32, f"vg{i}"), S.buf(f"vg{i}")) for i in range(2)])
        vln = Rot([(sb(C, ph, [128, 512], BF16, f"vln{i}"), S.buf(f"vln{i}")) for i in range(2)])
        stat = (sb(C, ph, [128, 2, 6], F32, "st6"), sb(C, ph, [128, 2], F32, "mv"), sb(C, ph, [128, 1], F32, "rstd")); Bstat = S.buf("stat")
        mtmp = sb(C, ph, [128, 4, 128], F32, "mtmp"); Bmtmp = S.buf("mtmp")
        bra = Rot([(sb(C, ph, [128, 4, 512], BF16, f"bra{i}"), S.buf(f"bra{i}")) for i in range(2)])
        PSA = Rot(PS[0:2]); PSV = Rot(PS[2:4]); PSM = Rot(PS[4:6])
        for i in range(NBLK):
            xb, Bxb = xbr.next()
            load_xblock(C, d, i, xs, Bxs, xb, Bxb)
            for g in range(4):
                pt, Bp = proj_fm(C, PSA, xb, Bxb, wb, Bw, g * 128)
                S.op("act", lambda e, g=g, pt=pt: e.activation(out=uT[:, g, :], in_=pt[:], func=AF.Gelu_apprx_tanh), reads=[Bp], writes=[BuT])
            brat, Bbra = bra.next()
            for c4 in range(4):
                pt, Bp = proj_tm(C, PSV, xb, Bxb, wb, Bw, 512, c4 * 128)
                vgt, Bvg = vg.next()
                S.op("act", lambda e, pt=pt, vgt=vgt: e.activation(out=vgt[:], in_=pt[:], func=AF.Gelu_apprx_tanh), reads=[Bp], writes=[Bvg])
                vl, Bvl = vln.next()
                layer_norm_tm(C, "pool", vgt[:], Bvg, vl[:], Bvl, g_rep[:], b_rep[:], Bsm, stat, Bstat, 512)
                pm, Bpm = PSM.next()
                for g in range(4):
                    S.op("pe", lambda e, g=g, vl=vl, pm=pm: e.matmul(pm[:, g * 128:(g + 1) * 128], lhsT=vl[:, g * 128:(g + 1) * 128], rhs=wsb[:, g, :], start=True, stop=True),
                         reads=[Bvl, Bwsb], writes=[Bpm], same_ok=True)
                S.op("dve", lambda e, pm=pm: e.tensor_tensor(out=mtmp[:].rearrange("p a b -> p (a b)"), in0=pm[:], in1=bs_rep[:].rearrange("p a b -> p (a b)"), op=ALU.add), reads=[Bpm, Bsm], writes=[Bmtmp])
                S.op("pool", lambda e, c4=c4, brat=brat: e.tensor_tensor(out=brat[:, :, c4 * 128:(c4 + 1) * 128], in0=mtmp[:], in1=uT[:, :, c4 * 128:(c4 + 1) * 128], op=ALU.mult), reads=[Bmtmp, BuT], writes=[Bbra])
            S.dma("sp", lambda e, i=i, brat=brat: e.dma_start(out=d["braT"][:, i * 512:(i + 1) * 512].rearrange("(g p) m -> p g m", p=128), in_=brat[:]), Bbra, reads=[Bbra])
        C.outbufs += [it[1] for it in bra.items]
        S.barrier()


def phase_A_ssm(C, d, consts, PS):
    S = C.S
    with ExitStack() as ph:
        tb = ssm_tables(C, ph, d, consts, PS)
        sbf = ssm_state_bufs(C, ph)
        stage = Rot([(sb(C, ph, [128, 2048], F32, f"wst{i}"), S.buf(f"wst{i}")) for i in range(2)])
        wb, Bw = load_w_bf16(C, ph, d["w_in"], 1024, 512, "wA2", stage)
        Bsm = S.buf("smallA2")
        dcol = sb(C, ph, [128, 4], F32, "dcol")
        S.dma("sp", lambda e: e.dma_start(out=dcol[:], in_=d["d_t"]), Bsm, writes=[Bsm])
        xs = sb(C, ph, [128, 4, 512], F32, "xs"); Bxs = S.buf("xs")
        xbr = Rot([(sb(C, ph, [128, 8, 512], BF16, f"xb{i}"), S.buf(f"xb{i}")) for i in range(1)])
        ubf = Rot([(sb(C, ph, [128, 4, 512], BF16, f"ubf{i}"), S.buf(f"ubf{i}")) for i in range(2)])
        uf = Rot([(sb(C, ph, [128, 4, 512], F32, f"uf{i}"), S.buf(f"uf{i}")) for i in range(1)])
        yl = Rot([(sb(C, ph, [128, 4, 512], F32, f"yl{i}"), S.buf(f"yl{i}")) for i in range(1)])
        aend = Rot([(sb(C, ph, [128, 16, 2], F32, f"aend{i}"), S.buf(f"aend{i}")) for i in range(2)])
        PSA = Rot(PS[0:2]); PSB = Rot(PS[2:6]); PSC, BPSC = PS[7]
        for i in range(NBLK):
            xb, Bxb = xbr.next()
            load_xblock(C, d, i, xs, Bxs, xb, Bxb)
            ub, Bub = ubf.next(); uff, Buf_ = uf.next()
            for g in range(4):
                pt, Bp = proj_fm(C, PSA, xb, Bxb, wb, Bw, g * 128)
                S.op("act", lambda e, g=g, pt=pt, ub=ub: e.copy(out=ub[:, g, :], in_=pt[:]), reads=[Bp], writes=[Bub])
                S.op("act", lambda e, g=g, pt=pt, uff=uff: e.copy(out=uff[:, g, :], in_=pt[:]), reads=[Bp], writes=[Buf_])
            ylt, Byl = yl.next()
            for c4 in range(4):
                ssm_local_tile(C, tb, sbf, ub, Bub, c4 * 128, c4 == 0, PSB, PSC, BPSC)
                for ct in range(4):
                    S.op("dve", lambda e, ct=ct, c4=c4, uff=uff, ylt=ylt: e.scalar_tensor_tensor(out=ylt[:, ct, c4 * 128:(c4 + 1) * 128], in0=uff[:, ct, c4 * 128:(c4 + 1) * 128], scalar=dcol[:, ct:ct + 1], in1=PSC[:, ct * 128:(ct + 1) * 128], op0=ALU.mult, op1=ALU.add),
                         reads=[Buf_, Bsm, BPSC], writes=[Byl])
                if c4 < 3:
                    ssm_next_init(C, tb, sbf, tb["CT"], tb["Bct"], 0, sbf["init"], sbf["Binit"])
                else:
                    ae, Bae = aend.next()
                    ssm_next_init(C, tb, sbf, tb["CS"], tb["Bcs"], 127, ae, Bae)
                    S.dma("sp", lambda e, i=i, ae=ae: e.dma_start(out=d["aend"][i], in_=ae[:]), Bae, reads=[Bae])
            S.dma("sp", lambda e, i=i, ylt=ylt: e.dma_start(out=d["ylocT"][:, i * 512:(i + 1) * 512].rearrange("(g p) m -> p g m", p=128), in_=ylt[:]), Byl, reads=[Byl])
        C.outbufs += [it[1] for it in yl.items + aend.items]
        S.barrier()


def core_token_index(c):
    r = c % 4
    idx = np.concatenate([np.arange((4 * i + r) * 512, (4 * i + r + 1) * 512) for i in range(8)])
    return c // 4, idx


def host_consts():
    o = {}
    o["ident"] = np.eye(128, dtype=np.float32)
    o["ones"] = np.ones((128, 128), np.float32)
    o["taus"] = np.tile(np.arange(128, dtype=np.float32), (128, 1))
    o["taus1"] = o["taus"] + 1.0
    o["t128"] = np.full((128, 1), 128.0, np.float32)
    s = np.arange(128)
    o["trimask"] = (s[:, None] <= s[None, :]).astype(np.float32)
    return o


def host_layer_params(inp, l):
    f = np.float32
    o = {}
    o["w_in"] = np.ascontiguousarray(inp["w_in"][l])
    o["sgu_g_rep"] = np.ascontiguousarray(np.tile(inp["sgu_ln_g"][l][None, :], (128, 1)))
    o["sgu_b_rep"] = np.ascontiguousarray(np.tile(inp["sgu_ln_b"][l][None, :], (128, 1)))
    o["sgu_wT"] = np.ascontiguousarray(inp["sgu_w"][l].transpose(2, 0, 1))
    o["sgu_bs_rep"] = np.ascontiguousarray(np.tile(inp["sgu_b"][l][None, :, :], (128, 1, 1)))

    def st_layout(a):
        return np.ascontiguousarray(a.reshape(16, 2, 64).transpose(1, 2, 0).reshape(128, 16))
    o["lamre"] = st_layout(inp["ssm_lambda_re"][l])
    o["lamim"] = st_layout(inp["ssm_lambda_im"][l])
    o["ldt"] = st_layout(np.tile(inp["ssm_log_dt"][l][:, None], (1, 64)))
    Bt_re = np.zeros((128, 16, 128), f); Bt_im = np.zeros((128, 16, 128), f)
    Ct_re = np.zeros((128, 16, 128), f); Ct_im = np.zeros((128, 16, 128), f)
    for g in range(32):
        st, gg = g // 2, g % 2
        r0 = 16 * (g % 8)
        Bt_re[r0:r0 + 16, st, gg * 64:(gg + 1) * 64] = inp["ssm_b_re"][l, g].T
        Bt_im[r0:r0 + 16, st, gg * 64:(gg + 1) * 64] = inp["ssm_b_im"][l, g].T
        Ct_re[gg * 64:(gg + 1) * 64, st, r0:r0 + 16] = inp["ssm_c_re"][l, g].T
        Ct_im[gg * 64:(gg + 1) * 64, st, r0:r0 + 16] = inp["ssm_c_im"][l, g].T
    o.update(Bt_re=Bt_re, Bt_im=Bt_im, Ct_re=Ct_re, Ct_im=Ct_im)
    o["d_t"] = np.ascontiguousarray(inp["ssm_d"][l].reshape(4, 128).T)
    return o


DN_ALPHA = (2 * 2) ** 0.25


def phase_A_qkvg(C, d, consts, PS):
    S = C.S
    with ExitStack() as ph:
        stage = Rot([(sb(C, ph, [128, 2048], F32, f"wst{i}"), S.buf(f"wst{i}")) for i in range(2)])
        wb, Bw = load_w_bf16(C, ph, d["w_in"], 1536, 4608, "wA3", stage)
        xs = sb(C, ph, [128, 4, 512], F32, "xs"); Bxs = S.buf("xs")
        xbr = Rot([(sb(C, ph, [128, 8, 512], BF16, f"xb{i}"), S.buf(f"xb{i}")) for i in range(2)])
        qk = Rot([(sb(C, ph, [128, 8, 512], BF16, f"qk{i}"), S.buf(f"qk{i}")) for i in range(2)])
        gs = Rot([(sb(C, ph, [128, 8, 512], BF16, f"gs{i}"), S.buf(f"gs{i}")) for i in range(3)])
        vs = Rot([(sb(C, ph, [128, 4, 512], BF16, f"vs{i}"), S.buf(f"vs{i}")) for i in range(2)])
        PSA = Rot(PS[0:4]); PSV = Rot(PS[4:6])
        for i in range(NBLK):
            xb, Bxb = xbr.next()
            load_xblock(C, d, i, xs, Bxs, xb, Bxb)
            qkt, Bqk = qk.next()
            for g in range(8):
                pt, Bp = proj_fm(C, PSA, xb, Bxb, wb, Bw, g * 128)
                if g % 2 == 0:
                    S.op("dve", lambda e, g=g, pt=pt, qkt=qkt: e.tensor_copy(out=qkt[:, g, :], in_=pt[:]), reads=[Bp], writes=[Bqk])
                else:
                    S.op("act", lambda e, g=g, pt=pt, qkt=qkt: e.copy(out=qkt[:, g, :], in_=pt[:]), reads=[Bp], writes=[Bqk])
            S.dma("sp", lambda e, i=i, qkt=qkt: e.dma_start(out=d["qT"][:, i * 512:(i + 1) * 512].rearrange("(g p) m -> p g m", p=128), in_=qkt[:, 0:4, :]), Bqk, reads=[Bqk])
            S.dma("sp", lambda e, i=i, qkt=qkt: e.dma_start(out=d["kT"][:, i * 512:(i + 1) * 512].rearrange("(g p) m -> p g m", p=128), in_=qkt[:, 4:8, :]), Bqk, reads=[Bqk])
            vst, Bvs = vs.next()
            for c4 in range(4):
                pt, Bp = proj_tm(C, PSV, xb, Bxb, wb, Bw, 1024, c4 * 128)
                S.op("dve", lambda e, c4=c4, pt=pt, vst=vst: e.tensor_copy(out=vst[:, c4, :], in_=pt[:]), reads=[Bp], writes=[Bvs])
            S.dma("sp", lambda e, i=i, vst=vst: e.dma_start(out=d["v"][i * 512:(i + 1) * 512, :].rearrange("(c p) m -> p c m", p=128), in_=vst[:]), Bvs, reads=[Bvs])
            for g3 in range(3):
                gst, Bgs = gs.next()
                for g in range(8):
                    pt, Bp = proj_fm(C, PSA, xb, Bxb, wb, Bw, 1536 + (g3 * 8 + g) * 128)
                    S.op("act", lambda e, g=g, pt=pt, gst=gst: e.activation(out=gst[:, g, :], in_=pt[:], func=AF.Sigmoid), reads=[Bp], writes=[Bgs])
                S.dma("sp", lambda e, i=i, g3=g3, gst=gst: e.dma_start(out=d["gatesT"][g3 * 1024:(g3 + 1) * 1024, i * 512:(i + 1) * 512].rearrange("(g p) m -> p g m", p=128), in_=gst[:]), Bgs, reads=[Bgs])
        C.outbufs += [it[1] for it in qk.items + gs.items + vs.items]
        S.barrier()


def ln_residual_tm(C, ps_halves, Bps, xres, Bxres, out, Bout, g_rep, b_rep, Bgb, tmp, Btmp, stat, Bstat):
    S = C.S
    for h in range(2):
        S.op("dve", lambda e, h=h: e.scalar_tensor_tensor(out=tmp[:, h * 512:(h + 1) * 512], in0=xres[:, h * 512:(h + 1) * 512], scalar=DN_ALPHA, in1=ps_halves[h], op0=ALU.mult, op1=ALU.add),
             reads=[Bxres, Bps[h]], writes=[Btmp])
    layer_norm_tm(C, "pool", tmp, Btmp, out, Bout, g_rep, b_rep, Bgb, stat, Bstat, 1024)


def phase_C1(C, d, consts, PS, l):
    S = C.S
    with ExitStack() as ph:
        stage = Rot([(sb(C, ph, [128, 2048], F32, f"wst{i}"), S.buf(f"wst{i}")) for i in range(2)])
        wbr = []
        for nm in ("w_a", "w_b", "w_c"):
            wbr.append(load_w_bf16(C, ph, d[nm], 0, 1024, nm + "b", stage, kchunks=4))
        wo, Bwo = load_w_bf16(C, ph, d["w_out"], 0, 1024, "wob", stage)
        g_rep = sb(C, ph, [128, 1024], F32, "ln1g"); b_rep = sb(C, ph, [128, 1024], F32, "ln1b"); Bgb = S.buf("ln1gb")
        S.dma("sp", lambda e: e.dma_start(out=g_rep[:], in_=d["ln1_g_rep"]), Bgb, writes=[Bgb])
        S.dma("sp", lambda e: e.dma_start(out=b_rep[:], in_=d["ln1_b_rep"]), Bgb, writes=[Bgb])
        brs = [Rot([(sb(C, ph, [128, 4, 512], BF16, f"br{k}_{i}"), S.buf(f"br{k}_{i}")) for i in range(2)]) for k in range(3)]
        gt = Rot([(sb(C, ph, [128, 24, 512], BF16, f"gt{i}"), S.buf(f"gt{i}")) for i in range(2)])
        xt = Rot([(sb(C, ph, [128, 4, 1024], F32, f"xt{i}"), S.buf(f"xt{i}")) for i in range(1)])
        mT = sb(C, ph, [128, 8, 512], BF16, "mT"); BmT = S.buf("mT")
        macc = Rot([(sb(C, ph, [128, 512], F32, f"macc{i}"), S.buf(f"macc{i}")) for i in range(2)])
        mt2 = Rot([(sb(C, ph, [128, 512], F32, f"mt2{i}"), S.buf(f"mt2{i}")) for i in range(2)])
        tmp = Rot([(sb(C, ph, [128, 1024], F32, f"lt{i}"), S.buf(f"lt{i}")) for i in range(2)])
        x1 = Rot([(sb(C, ph, [128, 1024], F32, f"x1{i}"), S.buf(f"x1{i}")) for i in range(2)])
        x1T = Rot([(sb(C, ph, [128, 8, 512], BF16, f"x1T{i}"), S.buf(f"x1T{i}")) for i in range(2)])
        stat = (sb(C, ph, [128, 2, 6], F32, "st6"), sb(C, ph, [128, 2], F32, "mv"), sb(C, ph, [128, 1], F32, "rstd")); Bstat = S.buf("stat")
        PSA = Rot(PS[0:3]); PSO = Rot(PS[3:7]); PST = Rot(PS[7:8])
        srcs = ("braT", "brbT", "brcT")
        for i in range(NBLK):
            brt = []
            for k in range(3):
                t, B = brs[k].next()
                S.dma("sp", lambda e, i=i, k=k, t=t: e.dma_start(out=t[:], in_=d[srcs[k]][:, i * 512:(i + 1) * 512].rearrange("(g p) m -> p g m", p=128)), B, reads=([C.Bbrb_dram] if k == 1 else []), writes=[B])
                brt.append((t, B))
            g_, Bg = gt.next()
            S.dma("sp", lambda e, i=i, g_=g_: e.dma_start(out=g_[:], in_=d["gatesT"][:, i * 512:(i + 1) * 512].rearrange("(g p) m -> p g m", p=128)), Bg, writes=[Bg])
            x_, Bx = xt.next()
            S.dma("sp", lambda e, i=i, x_=x_: e.dma_start(out=x_[:], in_=d["x_tok"][i * 512:(i + 1) * 512, :].rearrange("(c p) m -> p c m", p=128)), Bx, writes=[Bx])
            for dt_ in range(8):
                ma, Bma = macc.next()
                for k in range(3):
                    pt, Bp = proj_fm(C, PSA, brt[k][0], brt[k][1], wbr[k][0], wbr[k][1], dt_ * 128, kchunks=4)
                    if k == 0:
                        S.op("dve", lambda e, pt=pt, ma=ma, g_=g_, k=k, dt_=dt_: e.tensor_tensor(out=ma[:], in0=pt[:], in1=g_[:, k * 8 + dt_, :], op=ALU.mult), reads=[Bp, Bg], writes=[Bma])
                    else:
                        m2, Bm2 = mt2.next()
                        S.op("dve", lambda e, pt=pt, m2=m2, g_=g_, k=k, dt_=dt_: e.tensor_tensor(out=m2[:], in0=pt[:], in1=g_[:, k * 8 + dt_, :], op=ALU.mult), reads=[Bp, Bg], writes=[Bm2])
                        if k == 1:
                            S.op("pool", lambda e, ma=ma, m2=m2: e.tensor_tensor(out=ma[:], in0=ma[:], in1=m2[:], op=ALU.add), reads=[Bma, Bm2], writes=[Bma])
                        else:
                            S.op("pool", lambda e, ma=ma, m2=m2, dt_=dt_: e.tensor_tensor(out=mT[:, dt_, :], in0=ma[:], in1=m2[:], op=ALU.add), reads=[Bma, Bm2], writes=[BmT])
            x1Tt, Bx1T = x1T.next()
            for c4 in range(4):
                pss = []
                for h in range(2):
                    pt, Bp = proj_tm(C, PSO, mT, BmT, wo, Bwo, h * 512, c4 * 128)
                    pss.append((pt, Bp))
                tm, Btm = tmp.next()
                x1t, Bx1 = x1.next()
                ln_residual_tm(C, [pss[0][0][:], pss[1][0][:]], [pss[0][1], pss[1][1]], x_[:, c4, :], Bx, x1t[:], Bx1, g_rep[:], b_rep[:], Bgb, tm[:], Btm, stat, Bstat)
                S.dma("sp", lambda e, i=i, c4=c4, x1t=x1t: e.dma_start(out=d["x1_tok"][i * 512 + c4 * 128:i * 512 + (c4 + 1) * 128, :], in_=x1t[:]), Bx1, reads=[Bx1], writes=[C.Bx1_dram])
                for h in range(2):
                    pt, Bp = PST.next()
                    for k in range(4):
                        S.op("pe", lambda e, h=h, k=k, pt=pt, x1t=x1t: e.transpose(pt[:, k * 128:(k + 1) * 128], x1t[:, (h * 4 + k) * 128:(h * 4 + k + 1) * 128], consts["ident"][:]),
                             reads=[Bx1, consts["B"]], writes=[Bp], same_ok=True)
                    S.op("act", lambda e, h=h, c4=c4, pt=pt, x1Tt=x1Tt: e.copy(out=x1Tt[:, h * 4:(h + 1) * 4, c4 * 128:(c4 + 1) * 128], in_=pt[:].rearrange("p (a b) -> p a b", a=4)), reads=[Bp], writes=[Bx1T])
            S.dma("sp", lambda e, i=i, x1Tt=x1Tt: e.dma_start(out=d["x1T"][:, i * 512:(i + 1) * 512].rearrange("(g p) m -> p g m", p=128), in_=x1Tt[:]), Bx1T, reads=[Bx1T], writes=[C.Bx1T_dram])
        C.outbufs += [it[1] for it in x1.items + x1T.items]
        S.barrier()


def phase_C2(C, d, consts, PS, l, last):
    S = C.S
    with ExitStack() as ph:
        stage = Rot([(sb(C, ph, [128, 1024], F32, f"wst{i}"), S.buf(f"wst{i}")) for i in range(2)])
        w1, Bw1 = load_w_bf16_small(C, ph, d["ffn_w1"], 2816, "w1b", stage, 8)
        w3, Bw3 = load_w_bf16_small(C, ph, d["ffn_w3"], 2816, "w3b", stage, 8)
        w2, Bw2 = load_w_bf16_small(C, ph, d["ffn_w2"], 1024, "w2b", stage, 22)
        g_rep = sb(C, ph, [128, 1024], F32, "ln2g"); b_rep = sb(C, ph, [128, 1024], F32, "ln2b"); Bgb = S.buf("ln2gb")
        S.dma("sp", lambda e: e.dma_start(out=g_rep[:], in_=d["ln2_g_rep"]), Bgb, writes=[Bgb])
        S.dma("sp", lambda e: e.dma_start(out=b_rep[:], in_=d["ln2_b_rep"]), Bgb, writes=[Bgb])
        xT = Rot([(sb(C, ph, [128, 8, 512], BF16, f"fxT{i}"), S.buf(f"fxT{i}")) for i in range(1)])
        hT = sb(C, ph, [128, 22, 512], BF16, "hT"); BhT = [S.buf(f"hT{f}") for f in range(22)]
        sl = Rot([(sb(C, ph, [128, 512], F32, f"sl{i}"), S.buf(f"sl{i}")) for i in range(2)])
        xr = Rot([(sb(C, ph, [128, 1024], F32, f"xr{i}"), S.buf(f"xr{i}")) for i in range(1)])
        tmp = Rot([(sb(C, ph, [128, 1024], F32, f"lt{i}"), S.buf(f"lt{i}")) for i in range(1)])
        x2 = Rot([(sb(C, ph, [128, 1024], F32, f"x2{i}"), S.buf(f"x2{i}")) for i in range(2)])
        x2T = Rot([(sb(C, ph, [128, 4, 128], F32, f"x2T{i}"), S.buf(f"x2T{i}")) for i in range(2)])
        stat = (sb(C, ph, [128, 2, 6], F32, "st6"), sb(C, ph, [128, 2], F32, "mv"), sb(C, ph, [128, 1], F32, "rstd")); Bstat = S.buf("stat")
        PS1 = Rot(PS[0:2]); PS3 = Rot(PS[2:4]); PSO = Rot(PS[4:7]); PST = Rot(PS[7:8])
        for i in range(NBLK):
            xTt, BxT = xT.next()
            S.dma("sp", lambda e, i=i, xTt=xTt: e.dma_start(out=xTt[:], in_=d["x1T"][:, i * 512:(i + 1) * 512].rearrange("(g p) m -> p g m", p=128)), BxT, reads=[C.Bx1T_dram], writes=[BxT])
            for f in range(22):
                p1, Bp1 = proj_fm(C, PS1, xTt, BxT, w1, Bw1, f * 128)
                p3, Bp3 = proj_fm(C, PS3, xTt, BxT, w3, Bw3, f * 128)
                s_, Bs_ = sl.next()
                S.op("act", lambda e, p1=p1, s_=s_: e.activation(out=s_[:], in_=p1[:], func=AF.Silu), reads=[Bp1], writes=[Bs_])
                S.op("dve", lambda e, p3=p3, s_=s_, f=f: e.tensor_tensor(out=hT[:, f, :], in0=p3[:], in1=s_[:], op=ALU.mult), reads=[Bp3, Bs_], writes=[BhT[f]])
            for c4 in range(4):
                xr_, Bxr = xr.next()
                S.dma("sp", lambda e, i=i, c4=c4, xr_=xr_: e.dma_start(out=xr_[:], in_=d["x1_tok"][i * 512 + c4 * 128:i * 512 + (c4 + 1) * 128, :]), Bxr, reads=[C.Bx1_dram], writes=[Bxr])
                pss = []
                for h in range(2):
                    pt, Bp = PSO.next()
                    for f in range(22):
                        S.op("pe", lambda e, f=f, h=h, c4=c4, pt=pt: e.matmul(pt[:], lhsT=hT[:, f, c4 * 128:(c4 + 1) * 128], rhs=w2[:, f, h * 512:(h + 1) * 512], start=(f == 0), stop=(f == 21)),
                             reads=[BhT[f], Bw2], writes=[Bp], same_ok=True)
                    pss.append((pt, Bp))
                tm, Btm = tmp.next()
                x2t, Bx2 = x2.next()
                ln_residual_tm(C, [pss[0][0][:], pss[1][0][:]], [pss[0][1], pss[1][1]], xr_[:], Bxr, x2t[:], Bx2, g_rep[:], b_rep[:], Bgb, tm[:], Btm, stat, Bstat)
                S.dma("sp", lambda e, i=i, c4=c4, x2t=x2t: e.dma_start(out=d["out_tok"][i * 512 + c4 * 128:i * 512 + (c4 + 1) * 128, :], in_=x2t[:]), Bx2, reads=[Bx2], writes=[C.Bout_dram])
                if not last:
                    for h in range(2):
                        pt, Bp = PST.next()
                        for k in range(4):
                            S.op("pe", lambda e, h=h, k=k, pt=pt, x2t=x2t: e.transpose(pt[:, k * 128:(k + 1) * 128], x2t[:, (h * 4 + k) * 128:(h * 4 + k + 1) * 128], consts["ident"][:]),
                                 reads=[Bx2, consts["B"]], writes=[Bp], same_ok=True)
                        xo, Bxo = x2T.next()
                        S.op("act", lambda e, pt=pt, xo=xo: e.copy(out=xo[:].rearrange("p a b -> p (a b)"), in_=pt[:]), reads=[Bp], writes=[Bxo])
                        S.dma("sp", lambda e, i=i, c4=c4, h=h, xo=xo: e.dma_start(out=d["outT"][h * 512:(h + 1) * 512, i * 512 + c4 * 128:i * 512 + (c4 + 1) * 128].rearrange("(g p) m -> p g m", p=128), in_=xo[:]), Bxo, reads=[Bxo], writes=[C.BoutT_dram])
        C.outbufs += [it[1] for it in x2.items + x2T.items]
        S.barrier()


def load_w_bf16_small(C, ph, w_dram, ncols, name, stage, kchunks):
    S = C.S
    wb = sb(C, ph, [128, kchunks, ncols], BF16, name)
    Bw = S.buf(name)
    n = 0
    for kc in range(kchunks):
        for c0 in range(0, ncols, 1024):
            cw = min(1024, ncols - c0)
            stg, Bst = stage.next()
            S.dma("sp", lambda e, kc=kc, c0=c0, cw=cw, stg=stg: e.dma_start(out=stg[:, :cw], in_=w_dram[kc * 128:(kc + 1) * 128, c0:c0 + cw]), Bst, writes=[Bst])
            if n % 2:
                S.op("act", lambda e, kc=kc, c0=c0, cw=cw, stg=stg: e.copy(out=wb[:, kc, c0:c0 + cw], in_=stg[:, :cw]), reads=[Bst], writes=[Bw])
            else:
                S.op("pool", lambda e, kc=kc, c0=c0, cw=cw, stg=stg: e.tensor_copy(out=wb[:, kc, c0:c0 + cw], in_=stg[:, :cw]), reads=[Bst], writes=[Bw])
            n += 1
    return wb, Bw


def phase_B_ssm(C, d, consts, PS):
    S = C.S
    with ExitStack() as ph:
        tb = ssm_tables(C, ph, d, consts, PS)
        sbf = ssm_state_bufs(C, ph)
        th = tb["th"]; Bth = tb["Bth"]
        t512 = sb(C, ph, [128, 1], F32, "t512"); Bt5 = S.buf("t512")
        S.op("pool", lambda e: e.memset(t512[:], 512.0), writes=[Bt5])
        ang = sb(C, ph, [128, 128], F32, "ang2"); angc = sb(C, ph, [128, 128], F32, "angc2")
        ti = sb(C, ph, [128, 128], I32, "tti2"); tf = sb(C, ph, [128, 128], F32, "ttf2")
        pool = (ang, angc, ti, tf, S.buf("ang2"), S.buf("angc2"), S.buf("ttmp2"))
        C5, _, Bc5 = trig_table(C, ph, th, Bth, "r512", 16, t512, pool, Bt5)
        r512 = sb(C, ph, [128, 16], F32, "r512"); lnr = sb(C, ph, [128, 16], F32, "lnr"); Br5 = S.buf("r512")
        S.op("act", lambda e: e.activation(out=lnr[:], in_=tb["r"][:], func=AF.Ln), reads=[tb["Bs"]], writes=[Br5])
        S.op("act", lambda e: e.activation(out=r512[:], in_=lnr[:], func=AF.Exp, scale=512.0), reads=[Br5], writes=[Br5])
        L5 = sb(C, ph, [128, 2, 16], F32, "L5")
        S.op("dve", lambda e: e.tensor_tensor(out=L5[:, 0, :], in0=r512[:], in1=C5[:, :, 0, 0], op=ALU.mult), reads=[Br5, Bc5], writes=[Br5])
        S.op("dve", lambda e: e.tensor_tensor(out=L5[:, 1, :], in0=r512[:], in1=C5[:, :, 1, 0], op=ALU.mult), reads=[Br5, Bc5], writes=[Br5])
        A = sb(C, ph, [128, 35, 16, 2], F32, "Aall"); BA = S.buf("Aall")
        S.dma("sp", lambda e: e.dma_start(out=A[:], in_=d["aend_all"].rearrange("j p s c -> p j s c")), BA, writes=[BA])
        Hs = sb(C, ph, [128, 35, 16, 2], F32, "Hs"); BHs = S.buf("Hs")
        S.op("pool", lambda e: e.memset(Hs[:, 0], 0.0), writes=[BHs])
        tt = sb(C, ph, [128, 4, 16], F32, "pt4"); Btt = S.buf("pt4")
        nblk_needed = 4 * (NBLK - 1) + 3 + 1
        for j in range(1, nblk_needed):
            hr = Hs[:, j - 1, :, 0]; hi = Hs[:, j - 1, :, 1]
            S.op("dve", lambda e, hr=hr: e.tensor_tensor(out=tt[:, 0, :], in0=L5[:, 0, :], in1=hr, op=ALU.mult), reads=[Br5, BHs], writes=[Btt])
            S.op("dve", lambda e, hi=hi: e.tensor_tensor(out=tt[:, 1, :], in0=L5[:, 1, :], in1=hi, op=ALU.mult), reads=[Br5, BHs], writes=[Btt])
            S.op("dve", lambda e, hr=hr: e.tensor_tensor(out=tt[:, 2, :], in0=L5[:, 1, :], in1=hr, op=ALU.mult), reads=[Br5, BHs], writes=[Btt])
            S.op("dve", lambda e, hi=hi: e.tensor_tensor(out=tt[:, 3, :], in0=L5[:, 0, :], in1=hi, op=ALU.mult), reads=[Br5, BHs], writes=[Btt])
            S.op("dve", lambda e: e.tensor_tensor(out=tt[:, 0, :], in0=tt[:, 0, :], in1=tt[:, 1, :], op=ALU.subtract), reads=[Btt], writes=[Btt])
            S.op("dve", lambda e: e.tensor_tensor(out=tt[:, 2, :], in0=tt[:, 2, :], in1=tt[:, 3, :], op=ALU.add), reads=[Btt], writes=[Btt])
            S.op("dve", lambda e, j=j: e.tensor_tensor(out=Hs[:, j, :, 0], in0=tt[:, 0, :], in1=A[:, j - 1, :, 0], op=ALU.add), reads=[Btt, BA], writes=[BHs])
            S.op("dve", lambda e, j=j: e.tensor_tensor(out=Hs[:, j, :, 1], in0=tt[:, 2, :], in1=A[:, j - 1, :, 1], op=ALU.add), reads=[Btt, BA], writes=[BHs])
        stage = Rot([(sb(C, ph, [128, 2048], F32, f"wst{i}"), S.buf(f"wst{i}")) for i in range(2)])
        gw, Bgw = load_w_bf16(C, ph, d["glu_w"], 0, 512, "gluw", stage, kchunks=4)
        gb = sb(C, ph, [128, 4], F32, "glub"); Bgb = S.buf("glub")
        S.dma("sp", lambda e: e.dma_start(out=gb[:], in_=d["glu_b_t"]), Bgb, writes=[Bgb])
        yl = Rot([(sb(C, ph, [128, 4, 512], F32, f"yl{i}"), S.buf(f"yl{i}")) for i in range(2)])
        yg = Rot([(sb(C, ph, [128, 4, 512], BF16, f"yg{i}"), S.buf(f"yg{i}")) for i in range(2)])
        sg = Rot([(sb(C, ph, [128, 512], BF16, f"sg{i}"), S.buf(f"sg{i}")) for i in range(2)])
        brb = Rot([(sb(C, ph, [128, 4, 512], BF16, f"brb{i}"), S.buf(f"brb{i}")) for i in range(2)])
        PSA = Rot(PS[0:2]); PSC, BPSC = PS[7]
        for i in range(NBLK):
            j = 4 * i + 3
            ylt, Byl = yl.next()
            S.dma("sp", lambda e, i=i, ylt=ylt: e.dma_start(out=ylt[:], in_=d["ylocT"][:, i * 512:(i + 1) * 512].rearrange("(g p) m -> p g m", p=128)), Byl, writes=[Byl])
            it, Bit = sbf["itmp"], sbf["Bitmp"]
            c1 = tb["CS"][:, :, 0, 1]; s1 = tb["CS"][:, :, 1, 1]
            hr = Hs[:, j, :, 0]; hi = Hs[:, j, :, 1]
            S.op("dve", lambda e, hr=hr: e.tensor_tensor(out=it[:, 0, :], in0=c1, in1=hr, op=ALU.mult), reads=[tb["Bcs"], BHs], writes=[Bit])
            S.op("dve", lambda e, hi=hi: e.tensor_tensor(out=it[:, 1, :], in0=s1, in1=hi, op=ALU.mult), reads=[tb["Bcs"], BHs], writes=[Bit])
            S.op("dve", lambda e, hr=hr: e.tensor_tensor(out=it[:, 2, :], in0=s1, in1=hr, op=ALU.mult), reads=[tb["Bcs"], BHs], writes=[Bit])
            S.op("dve", lambda e, hi=hi: e.tensor_tensor(out=it[:, 3, :], in0=c1, in1=hi, op=ALU.mult), reads=[tb["Bcs"], BHs], writes=[Bit])
            S.op("dve", lambda e: e.tensor_tensor(out=sbf["init"][:, :, 0], in0=it[:, 0, :], in1=it[:, 1, :], op=ALU.subtract), reads=[Bit], writes=[sbf["Binit"]])
            S.op("dve", lambda e: e.tensor_tensor(out=sbf["init"][:, :, 1], in0=it[:, 2, :], in1=it[:, 3, :], op=ALU.add), reads=[Bit], writes=[sbf["Binit"]])
            ygt, Byg = yg.next()
            for c4 in range(4):
                ssm_corr_tile(C, tb, sbf, PSC, BPSC)
                S.op("dve", lambda e, c4=c4, ylt=ylt: e.tensor_tensor(out=ylt[:, :, c4 * 128:(c4 + 1) * 128], in0=ylt[:, :, c4 * 128:(c4 + 1) * 128], in1=PSC[:].rearrange("p (a b) -> p a b", a=4), op=ALU.add), reads=[Byl, BPSC], writes=[Byl])
                if c4 < 3:
                    ssm_next_init(C, tb, sbf, tb["CT"], tb["Bct"], 0, sbf["init"], sbf["Binit"])
            S.op("act", lambda e, ylt=ylt, ygt=ygt: e.activation(out=ygt[:], in_=ylt[:], func=AF.Gelu_apprx_tanh), reads=[Byl], writes=[Byg])
            brbt, Bbrb = brb.next()
            for g in range(4):
                pt, Bp = proj_fm(C, PSA, ygt, Byg, gw, Bgw, g * 128, kchunks=4)
                sgt, Bsg = sg.next()
                S.op("act", lambda e, g=g, pt=pt, sgt=sgt: e.activation(out=sgt[:], in_=pt[:], func=AF.Sigmoid, bias=gb[:, g:g + 1], scale=1.0), reads=[Bp, Bgb], writes=[Bsg])
                S.op("pool", lambda e, g=g, sgt=sgt, ygt=ygt, brbt=brbt: e.tensor_tensor(out=brbt[:, g, :], in0=ygt[:, g, :], in1=sgt[:], op=ALU.mult), reads=[Bsg, Byg], writes=[Bbrb])
            S.dma("sp", lambda e, i=i, brbt=brbt: e.dma_start(out=d["brbT"][:, i * 512:(i + 1) * 512].rearrange("(g p) m -> p g m", p=128), in_=brbt[:]), Bbrb, reads=[Bbrb], writes=[C.Bbrb_dram])
        C.outbufs += [it_[1] for it_ in brb.items]
        S.barrier()


NJ = 32


def phase_B_attn(C, d, consts, PS):
    S = C.S
    SEQL = NJ * 512
    with ExitStack() as ph:
        T2 = sb(C, ph, [68, SEQL], BF16, "T2"); BT2 = S.buf("T2")
        QS = min(4096, SEQL)
        for q4 in range(0, SEQL, QS):
            S.dma("sp", lambda e, q4=q4: e.dma_start(out=T2[:, q4:q4 + QS], in_=d["T2"][:, q4:q4 + QS]), BT2, writes=[BT2])
        trib = sb(C, ph, [128, 128], BF16, "trib"); Btr = S.buf("trib")
        S.op("dve", lambda e: e.tensor_copy(out=trib[:], in_=consts["trimask"][:]), reads=[consts["B"]], writes=[Btr])
        kTh = sb(C, ph, [64, SEQL], BF16, "kTh"); BkT = S.buf("kTh")
        vh = sb(C, ph, [128, SEQL // 128, 65], BF16, "vh"); Bvh = S.buf("vh")
        S.op("pool", lambda e: e.memset(vh[:, :, 64:65], 1.0), writes=[Bvh])
        qTh = sb(C, ph, [64, SEQL], BF16, "qTh"); BqT = S.buf("qTh")
        q2r = Rot([(sb(C, ph, [68, 512], BF16, f"q2_{i}"), S.buf(f"q2_{i}")) for i in range(2)])
        kmf = sb(C, ph, [64, 64], F32, "kmf"); kmh = sb(C, ph, [64, 64], BF16, "kmh"); kml = sb(C, ph, [64, 64], BF16, "kml"); kmt = sb(C, ph, [64, 64], F32, "kmt"); Bkm = S.buf("km")
        gsb = Rot([(sb(C, ph, [128, 64], F32, f"gsb{i}"), S.buf(f"gsb{i}")) for i in range(2)])
        m8 = Rot([(sb(C, ph, [128, 8], F32, f"m8{i}"), S.buf(f"m8{i}")) for i in range(2)])
        selm = Rot([(sb(C, ph, [128, 64], F32, f"selm{i}"), S.buf(f"selm{i}")) for i in range(2)])
        PT = Rot([(sb(C, ph, [128, 512], BF16, f"PT{i}"), S.buf(f"PT{i}")) for i in range(4)])
        recrow = Rot([(sb(C, ph, [65, 512], F32, f"recrow{i}"), S.buf(f"recrow{i}")) for i in range(2)])
        bcs = Rot([(sb(C, ph, [64, 512], F32, f"bcs{i}"), S.buf(f"bcs{i}")) for i in range(2)])
        oT = Rot([(sb(C, ph, [128, 512], BF16, f"oT{i}"), S.buf(f"oT{i}")) for i in range(2)])
        PG, BPG = PS[0]; PTr, BPTr = PS[0]; PSS = Rot(PS[1:4]); POr = Rot(PS[4:6]); PBC = Rot(PS[6:8])
        nkm = SEQL // 256
        S.op("pool", lambda e: e.memset(kmf[:], 0.0), writes=[Bkm])

        def gating(hh, j, q2, Bq2):
            S.dma("sp", lambda e: e.dma_start(out=q2[64:68, :], in_=d["q2c"][hh, :, j * 512:(j + 1) * 512]), Bq2, writes=[Bq2])
            for c4 in range(4):
                qb = 2 * j + c4 // 2
                qc0 = j * 512 + c4 * 128
                S.op("pe", lambda e, qc0=qc0: e.matmul(PG[:, 0:64], lhsT=qTh[:, qc0:qc0 + 128], rhs=kmh[:], start=True, stop=False), reads=[BqT, Bkm], writes=[BPG], same_ok=True)
                S.op("pe", lambda e, qc0=qc0: e.matmul(PG[:, 0:64], lhsT=qTh[:, qc0:qc0 + 128], rhs=kml[:], start=False, stop=True), reads=[BqT, Bkm], writes=[BPG], same_ok=True)
                g_, Bg_ = gsb.next()
                S.op("pool", lambda e, g_=g_: e.memset(g_[:], -1e30), writes=[Bg_])
                if qb > 0:
                    S.op("dve", lambda e, g_=g_, qb=qb: e.tensor_copy(out=g_[:, 0:qb], in_=PG[:, 0:qb]), reads=[BPG], writes=[Bg_])
                m_, Bm_ = m8.next()
                S.op("dve", lambda e, g_=g_, m_=m_: e.max(out=m_[:], in_=g_[:]), reads=[Bg_], writes=[Bm_])
                s_, Bs_ = selm.next()
                S.op("dve", lambda e, g_=g_, m_=m_, s_=s_: e.tensor_scalar(out=s_[:], in0=g_[:], scalar1=m_[:, 2:3], scalar2=-1.0, op0=ALU.is_ge, op1=ALU.add), reads=[Bg_, Bm_], writes=[Bs_])
                S.op("dve", lambda e, s_=s_, qb=qb: e.memset(s_[:, qb:qb + 1], 0.0), reads=[Bs_], writes=[Bs_])
                S.op("pe", lambda e, s_=s_: e.transpose(PTr[0:64, 128:256], s_[:], consts["ident"][:]), reads=[Bs_, consts["B"], BPG], writes=[BPTr], same_ok=True)
                S.op("act", lambda e, c4=c4: e.activation(out=q2[0:64, c4 * 128:(c4 + 1) * 128], in_=PTr[0:64, 128:256], func=AF.Copy, scale=30000.0), reads=[BPTr], writes=[Bq2])

        def qk(j, t, q2, Bq2):
            tt = t - 4 * j
            c_lo = max(tt, 0)
            q0 = j * 512 + c_lo * 128
            ncol = 512 - c_lo * 128
            ps_, Bps_ = PSS.next()
            S.op("pe", lambda e: e.matmul(ps_[:, 0:ncol], lhsT=kTh[:, t * 128:(t + 1) * 128], rhs=qTh[:, q0:q0 + ncol], start=True, stop=False), reads=[BkT, BqT], writes=[Bps_], same_ok=True)
            S.op("pe", lambda e: e.matmul(ps_[:, 0:ncol], lhsT=T2[:, t * 128:(t + 1) * 128], rhs=q2[:, c_lo * 128:512], start=False, stop=True), reads=[BT2, Bq2], writes=[Bps_], same_ok=True)
            return (ps_, Bps_, tt, c_lo, ncol)

        def exp_pv(j, t, st_, po, Bpo):
            ps_, Bps_, tt, c_lo, ncol = st_
            p_, Bp_ = PT.next()
            S.op("act", lambda e: e.activation(out=p_[:, 0:ncol], in_=ps_[:, 0:ncol], func=AF.Exp, scale=0.125), reads=[Bps_], writes=[Bp_])
            if tt >= 0:
                S.op("pool", lambda e: e.tensor_tensor(out=p_[:, 0:128], in0=p_[:, 0:128], in1=trib[:], op=ALU.mult), reads=[Bp_, Btr], writes=[Bp_])
            S.op("pe", lambda e: e.matmul(po[0:65, c_lo * 128:512], lhsT=vh[:, t, :], rhs=p_[:, 0:ncol], start=(t == 0), stop=(t == 4 * j + 3)),
                 reads=[Bp_, Bvh], writes=[Bpo], same_ok=True)

        for hh in range(2):
            S.dma("sp", lambda e, hh=hh: e.dma_start(out=kTh[:], in_=d["kT_hp"][hh * 64:(hh + 1) * 64, :]), BkT, writes=[BkT])
            for q4 in range(0, SEQL, QS):
                S.dma("sp", lambda e, hh=hh, q4=q4: e.dma_start(out=vh[:, q4 // 128:(q4 + QS) // 128, 0:64], in_=d["v_hp"][q4:q4 + QS, hh * 64:(hh + 1) * 64].rearrange("(t p) m -> p t m", p=128)), Bvh, writes=[Bvh])
            S.dma("sp", lambda e, hh=hh: e.dma_start(out=qTh[:], in_=d["qT_hp"][hh * 64:(hh + 1) * 64, :]), BqT, writes=[BqT])
            S.op("dve", lambda e: e.tensor_reduce(out=kmf[:, 0:nkm], in_=kTh[:].rearrange("p (n k) -> p n k", k=256), op=ALU.add, axis=AX.X), reads=[BkT], writes=[Bkm])
            S.op("dve", lambda e: e.tensor_scalar(out=kmf[:], in0=kmf[:], scalar1=1.0 / 256, scalar2=None, op0=ALU.mult), reads=[Bkm], writes=[Bkm])
            S.op("dve", lambda e: e.tensor_copy(out=kmh[:], in_=kmf[:]), reads=[Bkm], writes=[Bkm])
            S.op("dve", lambda e: e.tensor_copy(out=kmt[:], in_=kmh[:]), reads=[Bkm], writes=[Bkm])
            S.op("dve", lambda e: e.tensor_tensor(out=kmt[:], in0=kmf[:], in1=kmt[:], op=ALU.subtract), reads=[Bkm], writes=[Bkm])
            S.op("dve", lambda e: e.tensor_copy(out=kml[:], in_=kmt[:]), reads=[Bkm], writes=[Bkm])
            q2s = [q2r.next() for _ in range(NJ)]
            gating(hh, 0, *q2s[0])
            for j in range(NJ):
                q2, Bq2 = q2s[j]
                ntile = 4 * j + 4
                cur = qk(j, 0, q2, Bq2)
                if j + 1 < NJ:
                    gating(hh, j + 1, *q2s[j + 1])
                po, Bpo = POr.next()
                for t in range(ntile):
                    nxt = qk(j, t + 1, q2, Bq2) if t + 1 < ntile else None
                    exp_pv(j, t, cur, po, Bpo)
                    cur = nxt
                rr, Brr = recrow.next()
                S.op("dve", lambda e, po=po, rr=rr: e.reciprocal(out=rr[64:65, :], in_=po[64:65, :]), reads=[Bpo], writes=[Brr])
                pb, Bpb = PBC.next()
                S.op("pe", lambda e, rr=rr, pb=pb: e.matmul(pb[0:64, :], lhsT=consts["ones"][64:65, 0:64], rhs=rr[64:65, :], start=True, stop=True), reads=[Brr, consts["B"]], writes=[Bpb], same_ok=True)
                bs_, Bbs_ = bcs.next()
                S.op("act", lambda e, pb=pb, bs_=bs_: e.copy(out=bs_[0:64, :], in_=pb[0:64, :]), reads=[Bpb], writes=[Bbs_])
                o_, Bo_ = oT.next()
                S.op("dve", lambda e, po=po, bs_=bs_, o_=o_: e.tensor_tensor(out=o_[0:64, :], in0=po[0:64, :], in1=bs_[0:64, :], op=ALU.mult), reads=[Bpo, Bbs_], writes=[Bo_])
                S.dma("sp", lambda e, j=j, hh=hh, o_=o_: e.dma_start(out=d["brcT_hp"][hh * 64:(hh + 1) * 64, j * 512:(j + 1) * 512], in_=o_[0:64, :]), Bo_, reads=[Bo_])
        C.outbufs += [it_[1] for it_ in oT.items]
        S.barrier()


import ml_dtypes
from concourse.bass_utils import run_bass_kernel_spmd

NPBF = ml_dtypes.bfloat16
CONST_SHAPES = {"ident": [128, 128], "ones": [128, 128], "taus": [128, 128], "taus1": [128, 128], "t128": [128, 1], "trimask": [128, 128]}
SSM_SHAPES = {"lamre": [128, 16], "lamim": [128, 16], "ldt": [128, 16], "Bt_re": [128, 16, 128], "Bt_im": [128, 16, 128],
              "Ct_re": [128, 16, 128], "Ct_im": [128, 16, 128], "d_t": [128, 4]}
A_PARAM_SHAPES = dict({"w_in": [1024, 6144], "sgu_g_rep": [128, 512], "sgu_b_rep": [128, 512], "sgu_wT": [128, 4, 128], "sgu_bs_rep": [128, 4, 128]}, **SSM_SHAPES)
A_OUT_SHAPES = {"braT": ([512, NT], BF16), "ylocT": ([512, NT], F32), "aend": ([8, 128, 16, 2], F32), "qT": ([512, NT], BF16),
                "kT": ([512, NT], BF16), "v": ([NT, 512], BF16), "gatesT": ([3072, NT], BF16)}
C_PARAM_SHAPES = dict({"glu_w": [512, 512], "glu_b_t": [128, 4], "w_a": [512, 1024], "w_b": [512, 1024], "w_c": [512, 1024], "w_out": [1024, 1024],
                       "ln1_g_rep": [128, 1024], "ln1_b_rep": [128, 1024], "ffn_w1": [1024, 2816], "ffn_w3": [1024, 2816], "ffn_w2": [2816, 1024],
                       "ln2_g_rep": [128, 1024], "ln2_b_rep": [128, 1024]}, **SSM_SHAPES)


def _setup(nc, st):
    C = Ctx(nc, st)
    C.outbufs = []
    S = C.S
    C.eps = sb(C, st, [128, 1], F32, "eps")
    S.op("pool", lambda e: e.memset(C.eps[:], 1e-5), writes=[S.buf("eps")])
    for nm in ("BoutT_dram", "Bbrb_dram", "Bx1_dram", "Bx1T_dram", "Bout_dram"):
        setattr(C, nm, Buf(nm, multi=True))
    d = {}
    for k, shp in CONST_SHAPES.items():
        d[k] = C.dram_in(k, shp)
    cs = load_consts(C, st, d)
    PS = [(C.ps([128, 512]), S.buf(f"ps{i}")) for i in range(8)]
    return C, d, cs, PS


def build_A():
    nc = bass.Bass("TRN2", target_bir_lowering=False)
    with ExitStack() as st:
        C, d, cs, PS = _setup(nc, st)
        for k, shp in A_PARAM_SHAPES.items():
            d[k] = C.dram_in(k, shp)
        d["xT"] = C.dram_in("xT", [1024, NT])
        for k, (shp, dt_) in A_OUT_SHAPES.items():
            d[k] = C.dram_out(k, shp, dt_)
        phase_A_sgu(C, d, cs, PS)
        phase_A_ssm(C, d, cs, PS)
        phase_A_qkvg(C, d, cs, PS)
        C.S.finish("sp", C.outbufs)
        C.S.emit()
    return nc


def build_B():
    nc = bass.Bass("TRN2", target_bir_lowering=False)
    SEQL = NJ * 512
    with ExitStack() as st:
        C, d, cs, PS = _setup(nc, st)
        d["T2"] = C.dram_in("T2", [68, SEQL], BF16)
        d["q2c"] = C.dram_in("q2c", [2, 4, SEQL], BF16)
        d["qT_hp"] = C.dram_in("qT_hp", [128, SEQL], BF16)
        d["kT_hp"] = C.dram_in("kT_hp", [128, SEQL], BF16)
        d["v_hp"] = C.dram_in("v_hp", [SEQL, 128], BF16)
        d["brcT_hp"] = C.dram_out("brcT_hp", [128, SEQL], BF16)
        phase_B_attn(C, d, cs, PS)
        C.S.finish("sp", C.outbufs)
        C.S.emit()
    return nc


def build_C(last):
    nc = bass.Bass("TRN2", target_bir_lowering=False)
    with ExitStack() as st:
        C, d, cs, PS = _setup(nc, st)
        for k, shp in C_PARAM_SHAPES.items():
            d[k] = C.dram_in(k, shp)
        d["aend_all"] = C.dram_in("aend_all", [35, 128, 16, 2])
        d["ylocT"] = C.dram_in("ylocT", [512, NT])
        d["braT"] = C.dram_in("braT", [512, NT], BF16)
        d["brcT"] = C.dram_in("brcT", [512, NT], BF16)
        d["gatesT"] = C.dram_in("gatesT", [3072, NT], BF16)
        d["x_tok"] = C.dram_in("x_tok", [NT, 1024])
        d["brbT"] = C.dram_out("brbT", [512, NT], BF16)
        d["x1_tok"] = C.dram_out("x1_tok", [NT, 1024])
        d["x1T"] = C.dram_out("x1T", [1024, NT], BF16)
        d["out_tok"] = C.dram_out("out_tok", [NT, 1024])
        if not last:
            d["outT"] = C.dram_out("outT", [1024, NT])
        phase_B_ssm(C, d, cs, PS)
        phase_C1(C, d, cs, PS, 0)
        phase_C2(C, d, cs, PS, 0, last)
        if not last:
            dA = dict(d)
            for k, shp in A_PARAM_SHAPES.items():
                dA[k] = C.dram_in("n_" + k, shp)
            dA["xT"] = d["outT"]
            for k, (shp, dt_) in A_OUT_SHAPES.items():
                dA[k] = C.dram_out("n_" + k, shp, dt_)
            phase_A_sgu(C, dA, cs, PS)
            phase_A_ssm(C, dA, cs, PS)
            phase_A_qkvg(C, dA, cs, PS)
        C.S.finish("sp", C.outbufs + [C.Bout_dram, C.Bbrb_dram, C.Bx1_dram, C.Bx1T_dram, C.BoutT_dram])
        C.S.emit()
    return nc


def host_c_params(inp, l):
    o = {k: v for k, v in host_layer_params(inp, l).items() if k in SSM_SHAPES}
    o["glu_w"] = np.ascontiguousarray(inp["glu_w"][l])
    o["glu_b_t"] = np.ascontiguousarray(inp["glu_b"][l].reshape(4, 128).T)
    o["w_a"] = np.ascontiguousarray(inp["w_branch_a"][l]); o["w_b"] = np.ascontiguousarray(inp["w_branch_b"][l]); o["w_c"] = np.ascontiguousarray(inp["w_branch_c"][l])
    o["w_out"] = np.ascontiguousarray(inp["w_out"][l])
    for nm in ("ln1_g", "ln1_b", "ln2_g", "ln2_b"):
        o[nm + "_rep"] = np.ascontiguousarray(np.tile(inp[nm][l][None, :], (128, 1)))
    o["ffn_w1"] = np.ascontiguousarray(inp["ffn_w1"][l]); o["ffn_w3"] = np.ascontiguousarray(inp["ffn_w3"][l]); o["ffn_w2"] = np.ascontiguousarray(inp["ffn_w2"][l])
    return o


def host_attn_consts():
    SEQL = NJ * 512
    key = np.arange(SEQL)
    T2 = np.zeros((68, SEQL), np.float32)
    T2[key // 256, key] = 1.0
    T2[64] = key % 128; T2[65] = key // 128; T2[66] = 1.0; T2[67] = 1.0
    q2 = []
    for h in range(8):
        s8 = 8.0 * 2.0 ** (-(h + 1))
        q2.append(np.stack([np.full(SEQL, s8), np.full(SEQL, s8 * 128), -s8 * (key % 128), -s8 * 128 * (key // 128)]).astype(np.float32))
    return T2.astype(NPBF), np.stack(q2).astype(NPBF)


_PROGS = {}


def _prog(name, fn):
    if name not in _PROGS:
        _PROGS[name] = fn()
    return _PROGS[name]


def kernel(**inp):
    inp = {k: np.asarray(v) for k, v in inp.items()}
    x = inp["x"]
    consts = host_consts()
    T2, q2c_all = host_attn_consts()
    cores = list(range(8))
    tok = [core_token_index(c) for c in cores]
    x_tok = [np.ascontiguousarray(x[b][idx]) for b, idx in tok]

    def exchange_for_B(resA):
        maps = []
        full = {}
        for b in range(2):
            qT = np.zeros((512, 16384), NPBF); kT = np.zeros((512, 16384), NPBF); v = np.zeros((16384, 512), NPBF)
            for r in range(4):
                c = b * 4 + r
                idx = tok[c][1]
                qT[:, idx] = resA[c]["qT"]; kT[:, idx] = resA[c]["kT"]; v[idx, :] = resA[c]["v"]
            full[b] = (qT, kT, v)
        for c in cores:
            b, hp = c // 4, c % 4
            qT, kT, v = full[b]
            m = dict(consts)
            m["T2"] = T2
            m["q2c"] = np.ascontiguousarray(q2c_all[2 * hp:2 * hp + 2])
            m["qT_hp"] = np.ascontiguousarray(qT[hp * 128:(hp + 1) * 128]); m["kT_hp"] = np.ascontiguousarray(kT[hp * 128:(hp + 1) * 128])
            m["v_hp"] = np.ascontiguousarray(v[:, hp * 128:(hp + 1) * 128])
            maps.append(m)
        return maps

    def maps_for_C(l, resA, resB, xtoks, last):
        cp = host_c_params(inp, l)
        nextp = None if last else host_layer_params(inp, l + 1)
        maps = []
        for c in cores:
            b, r = c // 4, c % 4
            idx = tok[c][1]
            m = dict(consts); m.update(cp)
            A = np.zeros((35, 128, 16, 2), np.float32)
            for j in range(32):
                A[3 - r + j] = resA[b * 4 + j % 4]["aend"][j // 4]
            m["aend_all"] = A
            m["ylocT"] = resA[c]["ylocT"]; m["braT"] = resA[c]["braT"]; m["gatesT"] = resA[c]["gatesT"]
            brcT = np.concatenate([resB[b * 4 + hp]["brcT_hp"] for hp in range(4)], axis=0)
            m["brcT"] = np.ascontiguousarray(brcT[:, idx])
            m["x_tok"] = xtoks[c]
            if not last:
                for k, v_ in nextp.items():
                    m["n_" + k] = v_
            maps.append(m)
        return maps

    pA = host_layer_params(inp, 0)
    mapsA = []
    for c in cores:
        m = dict(consts); m.update(pA); m["xT"] = np.ascontiguousarray(x_tok[c].T)
        mapsA.append(m)
    resA = run_bass_kernel_spmd(_prog("A", build_A), mapsA, core_ids=cores).results
    resB = run_bass_kernel_spmd(_prog("B", build_B), exchange_for_B(resA), core_ids=cores).results
    res3 = run_bass_kernel_spmd(_prog("C0", lambda: build_C(False)), maps_for_C(0, resA, resB, x_tok, False), core_ids=cores).results
    resA1 = [{k: r_["n_" + k] for k in A_OUT_SHAPES} for r_ in res3]
    x1_tok = [np.asarray(r_["out_tok"]) for r_ in res3]
    resB1 = run_bass_kernel_spmd(_prog("B", build_B), exchange_for_B(resA1), core_ids=cores).results
    res5 = run_bass_kernel_spmd(_prog("C1", lambda: build_C(True)), maps_for_C(1, resA1, resB1, x1_tok, True), core_ids=cores).results
    out = np.zeros((2, 16384, 1024), np.float32)
    for c in cores:
        b, idx = tok[c]
        out[b, idx] = np.asarray(res5[c]["out_tok"])
    return out
```

```python
import numpy as np
import concourse.bass as bass
import concourse.mybir as mybir

F32 = mybir.dt.float32
BF16 = mybir.dt.bfloat16
I32 = mybir.dt.int32
AF = mybir.ActivationFunctionType
ALU = mybir.AluOpType
AX = mybir.AxisListType


class Buf:
    __slots__ = ("name", "lastw", "readers", "dsem", "dcount", "multi", "writers")

    def __init__(self, name, multi=False):
        self.name = name
        self.multi = multi
        self.writers = {}
        self.lastw = None
        self.readers = {}
        self.dsem = None
        self.dcount = 0


class Sched:
    ENG = ("pe", "dve", "act", "pool", "sp")

    def __init__(self, nc, stack):
        self.nc = nc
        self.stack = stack
        self.eng = {"pe": nc.tensor, "dve": nc.vector, "act": nc.scalar,
                    "pool": nc.gpsimd, "sp": nc.sync}
        self.ops = {e: [] for e in self.ENG}
        self.cnt = {e: 0 for e in self.ENG}
        self.sem = {}
        for e in ("pe", "dve", "act", "pool"):
            self.sem[e] = stack.enter_context(nc.semaphore("s_" + e))
        self.seen = {e: {} for e in self.ENG}
        self.nbuf = 0
        self.final_waits = []

    def buf(self, name=None):
        self.nbuf += 1
        return Buf(name or f"b{self.nbuf}")

    def bufs(self, n, name="b"):
        return [self.buf(f"{name}{i}") for i in range(n)]

    def _need(self, e, waits, key, val):
        if key is None:
            return
        if self.seen[e].get(key, 0) >= val:
            return
        waits[key] = max(waits.get(key, 0), val)

    def _deps(self, e, reads, writes, same_ok=False):
        waits = {}
        for b in reads:
            if b.multi:
                for k, v in b.writers.items():
                    self._need(e, waits, k, v)
            elif b.lastw is not None:
                k, v = b.lastw
                if not (same_ok and k == e):
                    self._need(e, waits, k, v)
        for b in writes:
            if b.multi:
                continue
            if b.lastw is not None:
                k, v = b.lastw
                if not (same_ok and k == e):
                    self._need(e, waits, k, v)
            for k, v in b.readers.items():
                if k == e:
                    continue
                self._need(e, waits, k, v)
        for k, v in waits.items():
            self.seen[e][k] = v
        return waits

    def op(self, e, fn, reads=(), writes=(), same_ok=False):
        waits = self._deps(e, reads, writes, same_ok)
        self.cnt[e] += 1
        n = self.cnt[e]
        self.ops[e].append((waits, fn, self.sem[e], 1))
        for b in reads:
            b.readers[e] = n
        for b in writes:
            b.lastw = (e, n)
            b.readers = {}

    def dma(self, q, fn, owner, reads=(), writes=()):
        if owner.dsem is None:
            self.nsem = getattr(self, "nsem", 0) + 1
            owner.dsem = self.stack.enter_context(self.nc.semaphore(f"d{self.nsem}_{owner.name}"))
            if not hasattr(self, "dma_owners"):
                self.dma_owners = []
            self.dma_owners.append(owner)
        waits = self._deps(q, reads, writes)
        key = owner
        waits.pop(key, None)
        owner.dcount += 16
        n = owner.dcount
        self.ops[q].append((waits, fn, owner.dsem, 16))
        for b in reads:
            b.readers[key] = n
        for b in writes:
            if b.multi:
                b.writers[key] = n
                continue
            b.lastw = (key, n)
            b.readers = {}

    def barrier(self):
        owners = getattr(self, "dma_owners", [])
        for e in self.ENG:
            waits = {}
            for k in ("pe", "dve", "act", "pool"):
                if k != e and self.cnt[k] > 0:
                    self._need(e, waits, k, self.cnt[k])
            for o in owners:
                if o.dcount > 0:
                    self._need(e, waits, o, o.dcount)
            for k, v in waits.items():
                self.seen[e][k] = v
            self.ops[e].append((waits, None, None, 0))

    def finish(self, e, bufs):
        waits = {}
        for b in bufs:
            for k, v in b.writers.items():
                self._need(e, waits, k, v)
            if b.lastw is not None:
                self._need(e, waits, *b.lastw)
            for k, v in b.readers.items():
                self._need(e, waits, k, v)
        self.ops[e].append((waits, None, None, 0))

    def _semof(self, key):
        if isinstance(key, Buf):
            return key.dsem
        return self.sem[key]

    def emit(self):
        nc = self.nc
        with nc.Block() as block:
            def mk(e):
                def body(engine):
                    for waits, fn, sem, inc in self.ops[e]:
                        for k, v in waits.items():
                            engine.wait_ge(self._semof(k), v)
                        if fn is not None:
                            fn(engine).then_inc(sem, inc)
                return body
            block.tensor(mk("pe"))
            block.vector(mk("dve"))
            block.scalar(mk("act"))
            block.gpsimd(mk("pool"))
            block.sync(mk("sp"))


class Ctx:
    def __init__(self, nc, stack):
        self.nc = nc
        self.st = stack
        self.S = Sched(nc, stack)
        self.n = 0
        self.rr = 0

    def sb(self, shape, dt, name=None):
        self.n += 1
        return self.st.enter_context(self.nc.sbuf_tensor(name or f"t{self.n}", list(shape), dt))

    def ps(self, shape, dt=F32, name=None):
        self.n += 1
        return self.st.enter_context(self.nc.psum_tensor(name or f"p{self.n}", list(shape), dt))

    def dram_in(self, name, shape, dt=F32):
        return self.nc.dram_tensor(name, list(shape), dt, kind="ExternalInput").ap()

    def dram_out(self, name, shape, dt=F32):
        return self.nc.dram_tensor(name, list(shape), dt, kind="ExternalOutput").ap()


import math
from contextlib import ExitStack

NT = 4096
NBLK = 8
TB = 512
T = 128
D = 1024
TWO_PI = 2 * math.pi


def sb(C, ph, shape, dt, name):
    C.n += 1
    return ph.enter_context(C.nc.sbuf_tensor(f"s{C.n}_{name}", list(shape), dt))


def range_reduce(C, eng, ang, tmpi, tmpf, Bang, Btmp):
    S = C.S
    S.op(eng, lambda e: e.tensor_scalar(out=tmpi, in0=ang, scalar1=1.0 / TWO_PI, scalar2=None, op0=ALU.mult), reads=[Bang], writes=[Btmp])
    S.op(eng, lambda e: e.tensor_copy(out=tmpf, in_=tmpi), reads=[Btmp], writes=[Btmp])
    S.op(eng, lambda e: e.scalar_tensor_tensor(out=ang, in0=tmpf, scalar=-TWO_PI, in1=ang, op0=ALU.mult, op1=ALU.add), reads=[Btmp, Bang], writes=[Bang])
    S.op(eng, lambda e: e.tensor_scalar(out=tmpf, in0=ang, scalar1=math.pi, scalar2=-TWO_PI, op0=ALU.is_gt, op1=ALU.mult), reads=[Bang], writes=[Btmp])
    S.op(eng, lambda e: e.tensor_tensor(out=ang, in0=ang, in1=tmpf, op=ALU.add), reads=[Bang, Btmp], writes=[Bang])
    S.op(eng, lambda e: e.tensor_scalar(out=tmpf, in0=ang, scalar1=-math.pi, scalar2=TWO_PI, op0=ALU.is_lt, op1=ALU.mult), reads=[Bang], writes=[Btmp])
    S.op(eng, lambda e: e.tensor_tensor(out=ang, in0=ang, in1=tmpf, op=ALU.add), reads=[Bang, Btmp], writes=[Bang])


def ssm_prep(C, ph, d):
    S = C.S
    nc = C.nc
    o = {}
    P16 = [128, 16]
    lamre = sb(C, ph, P16, F32, "lamre"); lamim = sb(C, ph, P16, F32, "lamim"); ldt = sb(C, ph, P16, F32, "ldt")
    Bp = S.buf("ssmp")
    S.dma("sp", lambda e: e.dma_start(out=lamre[:], in_=d["lamre"]), Bp, writes=[Bp])
    S.dma("sp", lambda e: e.dma_start(out=lamim[:], in_=d["lamim"]), Bp, writes=[Bp])
    S.dma("sp", lambda e: e.dma_start(out=ldt[:], in_=d["ldt"]), Bp, writes=[Bp])
    dt_ = sb(C, ph, P16, F32, "dt"); th = sb(C, ph, P16, F32, "th"); r = sb(C, ph, P16, F32, "r")
    ti = sb(C, ph, P16, I32, "ti"); tf = sb(C, ph, P16, F32, "tf"); a_ = sb(C, ph, P16, F32, "a_")
    Bs = S.buf("ssms"); Bth = S.buf("th"); Bt = S.buf("tmp")
    S.op("act", lambda e: e.activation(out=dt_[:], in_=ldt[:], func=AF.Exp), reads=[Bp], writes=[Bs])
    S.op("dve", lambda e: e.tensor_tensor(out=a_[:], in0=lamre[:], in1=dt_[:], op=ALU.mult), reads=[Bp, Bs], writes=[Bs])
    S.op("act", lambda e: e.activation(out=r[:], in_=a_[:], func=AF.Exp), reads=[Bs], writes=[Bs])
    S.op("dve", lambda e: e.tensor_tensor(out=th[:], in0=lamim[:], in1=dt_[:], op=ALU.mult), reads=[Bp, Bs], writes=[Bth])
    range_reduce(C, "dve", th[:], ti[:], tf[:], Bth, Bt)
    sn = sb(C, ph, P16, F32, "sn"); cs = sb(C, ph, P16, F32, "cs"); thc = sb(C, ph, P16, F32, "thc")
    Bc = S.buf("thc")
    S.op("act", lambda e: e.activation(out=sn[:], in_=th[:], func=AF.Sin), reads=[Bth], writes=[Bs])
    S.op("dve", lambda e: e.tensor_scalar(out=thc[:], in0=th[:], scalar1=math.pi / 2, scalar2=None, op0=ALU.add), reads=[Bth], writes=[Bc])
    range_reduce(C, "dve", thc[:], ti[:], tf[:], Bc, Bt)
    S.op("act", lambda e: e.activation(out=cs[:], in_=thc[:], func=AF.Sin), reads=[Bc], writes=[Bs])
    lbr = sb(C, ph, P16, F32, "lbr"); lbi = sb(C, ph, P16, F32, "lbi")
    S.op("dve", lambda e: e.tensor_tensor(out=lbr[:], in0=r[:], in1=cs[:], op=ALU.mult), reads=[Bs], writes=[Bs])
    S.op("dve", lambda e: e.tensor_tensor(out=lbi[:], in0=r[:], in1=sn[:], op=ALU.mult), reads=[Bs], writes=[Bs])
    nr = sb(C, ph, P16, F32, "nr"); den = sb(C, ph, P16, F32, "den"); t1 = sb(C, ph, P16, F32, "t1"); t2 = sb(C, ph, P16, F32, "t2")
    cr = sb(C, ph, P16, F32, "cr"); ci = sb(C, ph, P16, F32, "ci")
    S.op("dve", lambda e: e.tensor_scalar(out=nr[:], in0=lbr[:], scalar1=-1.0, scalar2=None, op0=ALU.add), reads=[Bs], writes=[Bs])
    S.op("dve", lambda e: e.tensor_tensor(out=t1[:], in0=lamre[:], in1=lamre[:], op=ALU.mult), reads=[Bp], writes=[Bs])
    S.op("dve", lambda e: e.tensor_tensor(out=t2[:], in0=lamim[:], in1=lamim[:], op=ALU.mult), reads=[Bp], writes=[Bs])
    S.op("dve", lambda e: e.tensor_tensor(out=den[:], in0=t1[:], in1=t2[:], op=ALU.add), reads=[Bs], writes=[Bs])
    S.op("dve", lambda e: e.reciprocal(out=den[:], in_=den[:]), reads=[Bs], writes=[Bs])
    S.op("dve", lambda e: e.tensor_tensor(out=t1[:], in0=nr[:], in1=lamre[:], op=ALU.mult), reads=[Bs, Bp], writes=[Bs])
    S.op("dve", lambda e: e.tensor_tensor(out=t2[:], in0=lbi[:], in1=lamim[:], op=ALU.mult), reads=[Bs, Bp], writes=[Bs])
    S.op("dve", lambda e: e.tensor_tensor(out=t1[:], in0=t1[:], in1=t2[:], op=ALU.add), reads=[Bs], writes=[Bs])
    S.op("dve", lambda e: e.tensor_tensor(out=cr[:], in0=t1[:], in1=den[:], op=ALU.mult), reads=[Bs], writes=[Bs])
    S.op("dve", lambda e: e.tensor_tensor(out=t1[:], in0=lbi[:], in1=lamre[:], op=ALU.mult), reads=[Bs, Bp], writes=[Bs])
    S.op("dve", lambda e: e.tensor_tensor(out=t2[:], in0=nr[:], in1=lamim[:], op=ALU.mult), reads=[Bs, Bp], writes=[Bs])
    S.op("dve", lambda e: e.tensor_tensor(out=t1[:], in0=t1[:], in1=t2[:], op=ALU.subtract), reads=[Bs], writes=[Bs])
    S.op("dve", lambda e: e.tensor_tensor(out=ci[:], in0=t1[:], in1=den[:], op=ALU.mult), reads=[Bs], writes=[Bs])
    o.update(th=th, r=r, lbr=lbr, lbi=lbi, cr=cr, ci=ci, Bs=Bs, Bth=Bth)
    return o


def trig_table(C, ph, th, Bth, name, n_st, taus, tmp_pool, Btaus):
    S = C.S
    n = taus.shape[1]
    CS = sb(C, ph, [128, n_st, 2, n], F32, name + "CS")
    SC = sb(C, ph, [128, n_st, 2, n], F32, name + "SC")
    B = S.buf(name)
    ang, angc, ti, tf, Ba, Bc, Bt = tmp_pool
    for st in range(n_st):
        S.op("dve", lambda e, st=st: e.tensor_scalar(out=ang[:, :n], in0=taus[:], scalar1=th[:, st:st + 1], scalar2=None, op0=ALU.mult), reads=[Bth, Btaus], writes=[Ba])
        range_reduce(C, "dve", ang[:, :n], ti[:, :n], tf[:, :n], Ba, Bt)
        S.op("act", lambda e, st=st: e.activation(out=CS[:, st, 1, :], in_=ang[:, :n], func=AF.Sin), reads=[Ba], writes=[B])
        S.op("dve", lambda e: e.tensor_scalar(out=angc[:, :n], in0=ang[:, :n], scalar1=math.pi / 2, scalar2=None, op0=ALU.add), reads=[Ba], writes=[Bc])
        range_reduce(C, "dve", angc[:, :n], ti[:, :n], tf[:, :n], Bc, Bt)
        S.op("act", lambda e, st=st: e.activation(out=CS[:, st, 0, :], in_=angc[:, :n], func=AF.Sin), reads=[Bc], writes=[B])
    S.op("pool", lambda e: e.tensor_copy(out=SC[:, :, 0, :], in_=CS[:, :, 1, :]), reads=[B], writes=[B])
    S.op("pool", lambda e: e.tensor_copy(out=SC[:, :, 1, :], in_=CS[:, :, 0, :]), reads=[B], writes=[B])
    return CS, SC, B


class Rot:
    def __init__(self, items):
        self.items = items
        self.i = 0

    def next(self):
        it = self.items[self.i % len(self.items)]
        self.i += 1
        return it


def ssm_tables(C, ph, d, consts, PS):
    S = C.S
    o = ssm_prep(C, ph, d)
    Bs, Bth = o["Bs"], o["Bth"]
    th = o["th"]
    ang = sb(C, ph, [128, 128], F32, "ang"); angc = sb(C, ph, [128, 128], F32, "angc")
    ti = sb(C, ph, [128, 128], I32, "tti"); tf = sb(C, ph, [128, 128], F32, "ttf")
    pool = (ang, angc, ti, tf, S.buf("ang"), S.buf("angc"), S.buf("ttmp"))
    CS, SC, Bcs = trig_table(C, ph, th, Bth, "rot", 16, consts["taus"], pool, consts["B"])
    CT, _, Bct = trig_table(C, ph, th, Bth, "rT", 16, consts["t128"], pool, consts["B"])
    o.update(CS=CS, SC=SC, Bcs=Bcs, CT=CT, Bct=Bct)
    R = sb(C, ph, [128, 16, 128], F32, "Rm"); Rp = sb(C, ph, [128, 16, 128], F32, "Rp")
    a_ = sb(C, ph, [128, 16], F32, "a2")
    BR = S.buf("R")
    S.op("act", lambda e: e.activation(out=a_[:], in_=o["r"][:], func=AF.Ln), reads=[Bs], writes=[BR])
    for st in range(16):
        S.op("pool", lambda e, st=st: e.tensor_scalar(out=R[:, st, :], in0=consts["ones"][:], scalar1=o["r"][:, st:st + 1], scalar2=None, op0=ALU.mult), reads=[Bs, consts["B"]], writes=[BR])
        S.op("act", lambda e, st=st: e.activation(out=Rp[:, st, :], in_=consts["taus1"][:], func=AF.Exp, scale=a_[:, st:st + 1]), reads=[BR, consts["B"]], writes=[BR])
    o.update(R=R, Rp=Rp, BR=BR)
    Bt_re = sb(C, ph, [128, 16, 128], F32, "Bt_re"); Bt_im = sb(C, ph, [128, 16, 128], F32, "Bt_im")
    Bld = S.buf("Btld")
    S.dma("sp", lambda e: e.dma_start(out=Bt_re[:], in_=d["Bt_re"]), Bld, writes=[Bld])
    S.dma("sp", lambda e: e.dma_start(out=Bt_im[:], in_=d["Bt_im"]), Bld, writes=[Bld])
    LB = sb(C, ph, [128, 16, 2, 128], BF16, "LB")
    BLB = S.buf("LB")
    Dm = Rot([(sb(C, ph, [128, 2, 128], F32, f"Dm{i}"), S.buf(f"Dm{i}")) for i in range(2)])
    tt = Rot([(sb(C, ph, [128, 4, 128], F32, f"tq{i}"), S.buf(f"tq{i}")) for i in range(2)])
    prot = Rot(PS[0:2])
    for st in range(16):
        Dt, BD = Dm.next()
        S.op("pool", lambda e, st=st, Dt=Dt: e.tensor_scalar(out=Dt[:, 0, :], in0=consts["ident"][:], scalar1=o["cr"][:, st:st + 1], scalar2=None, op0=ALU.mult), reads=[Bs, consts["B"]], writes=[BD])
        S.op("pool", lambda e, st=st, Dt=Dt: e.tensor_scalar(out=Dt[:, 1, :], in0=consts["ident"][:], scalar1=o["ci"][:, st:st + 1], scalar2=None, op0=ALU.mult), reads=[Bs, consts["B"]], writes=[BD])
        pt, Bpt = prot.next()
        S.op("pe", lambda e, Dt=Dt, pt=pt: e.matmul(pt[:, 0:256], lhsT=consts["ones"][:], rhs=Dt[:].rearrange("p a b -> p (a b)"), start=True, stop=True), reads=[BD, consts["B"]], writes=[Bpt])
        tq, Bq = tt.next()
        S.op("dve", lambda e, st=st, pt=pt, tq=tq: e.tensor_tensor(out=tq[:, 0, :], in0=pt[:, 0:128], in1=Bt_re[:, st, :], op=ALU.mult), reads=[Bpt, Bld], writes=[Bq])
        S.op("dve", lambda e, st=st, pt=pt, tq=tq: e.tensor_tensor(out=tq[:, 1, :], in0=pt[:, 128:256], in1=Bt_im[:, st, :], op=ALU.mult), reads=[Bpt, Bld], writes=[Bq])
        S.op("dve", lambda e, st=st, pt=pt, tq=tq: e.tensor_tensor(out=tq[:, 2, :], in0=pt[:, 0:128], in1=Bt_im[:, st, :], op=ALU.mult), reads=[Bpt, Bld], writes=[Bq])
        S.op("dve", lambda e, st=st, pt=pt, tq=tq: e.tensor_tensor(out=tq[:, 3, :], in0=pt[:, 128:256], in1=Bt_re[:, st, :], op=ALU.mult), reads=[Bpt, Bld], writes=[Bq])
        S.op("pool", lambda e, st=st, tq=tq: e.tensor_tensor(out=LB[:, st, 0, :], in0=tq[:, 0, :], in1=tq[:, 1, :], op=ALU.subtract), reads=[Bq], writes=[BLB])
        S.op("pool", lambda e, st=st, tq=tq: e.tensor_tensor(out=LB[:, st, 1, :], in0=tq[:, 2, :], in1=tq[:, 3, :], op=ALU.add), reads=[Bq], writes=[BLB])
    Ct_re = Bt_re; Ct_im = Bt_im
    LC = sb(C, ph, [128, 16, 2, 128], BF16, "LC")
    BLC = S.buf("LC")
    S.dma("sp", lambda e: e.dma_start(out=Ct_re[:], in_=d["Ct_re"]), Bld, reads=[BLB], writes=[Bld])
    S.dma("sp", lambda e: e.dma_start(out=Ct_im[:], in_=d["Ct_im"]), Bld, reads=[BLB], writes=[Bld])
    S.op("pool", lambda e: e.tensor_copy(out=LC[:, :, 0, :], in_=Ct_re[:]), reads=[Bld], writes=[BLC])
    S.op("pool", lambda e: e.tensor_scalar(out=LC[:, :, 1, :], in0=Ct_im[:], scalar1=-1.0, scalar2=None, op0=ALU.mult), reads=[Bld], writes=[BLC])
    o.update(LB=LB, BLB=BLB, LC=LC, BLC=BLC)
    return o


def ssm_state_bufs(C, ph):
    S = C.S
    o = {}
    o["GG"] = sb(C, ph, [128, 16, 2, 128], F32, "GG")
    o["BGG"] = [S.buf(f"GG{st}") for st in range(16)]
    o["H"] = sb(C, ph, [128, 16, 2, 128], BF16, "Hb")
    o["BH"] = [S.buf(f"H{st}") for st in range(16)]
    o["init"] = sb(C, ph, [128, 16, 2], F32, "ginit")
    o["Binit"] = S.buf("ginit")
    o["itmp"] = sb(C, ph, [128, 4, 16], F32, "itmp")
    o["Bitmp"] = S.buf("itmp")
    o["rt"] = Rot([(sb(C, ph, [128, 2, 2, 128], F32, f"rt{i}"), S.buf(f"rt{i}")) for i in range(3)])
    o["bp"] = Rot([(sb(C, ph, [128, 2, 128], F32, f"bp{i}"), S.buf(f"bp{i}")) for i in range(3)])
    o["pt"] = Rot([(sb(C, ph, [128, 2, 2, 128], F32, f"pq{i}"), S.buf(f"pq{i}")) for i in range(3)])
    return o


def ssm_rot_out_and_C(C, tb, sbf, st_list, PSC, BPSC):
    S = C.S
    GG, H = sbf["GG"], sbf["H"]
    CS, SC, Bcs = tb["CS"], tb["SC"], tb["Bcs"]
    for st in st_list:
        p12, Bp = sbf["pt"].next()
        S.op("pool", lambda e, st=st, p12=p12: e.tensor_tensor(out=p12[:, 0], in0=CS[:, st], in1=GG[:, st], op=ALU.mult), reads=[Bcs, sbf["BGG"][st]], writes=[Bp])
        S.op("pool", lambda e, st=st, p12=p12: e.tensor_tensor(out=p12[:, 1], in0=SC[:, st], in1=GG[:, st], op=ALU.mult), reads=[Bcs, sbf["BGG"][st]], writes=[Bp])
        S.op("pool", lambda e, st=st, p12=p12: e.tensor_tensor(out=H[:, st, 0, :], in0=p12[:, 0, 0, :], in1=p12[:, 0, 1, :], op=ALU.subtract), reads=[Bp], writes=[sbf["BH"][st]])
        S.op("pool", lambda e, st=st, p12=p12: e.tensor_tensor(out=H[:, st, 1, :], in0=p12[:, 1, 0, :], in1=p12[:, 1, 1, :], op=ALU.add), reads=[Bp], writes=[sbf["BH"][st]])
    LC, BLC = tb["LC"], tb["BLC"]
    for ct in range(4):
        n = 0
        for st in range(4 * ct, 4 * ct + 4):
            for ri in range(2):
                S.op("pe", lambda e, ct=ct, st=st, ri=ri, n=n: e.matmul(PSC[:, ct * 128:(ct + 1) * 128], lhsT=LC[:, st, ri, :], rhs=H[:, st, ri, :], start=(n == 0), stop=(n == 7)),
                     reads=[BLC, sbf["BH"][st]], writes=[BPSC], same_ok=True)
                n += 1


def ssm_next_init(C, tb, sbf, table, Btable, col, out, Bout):
    S = C.S
    GG = sbf["GG"]
    it, Bit = sbf["itmp"], sbf["Bitmp"]
    c = table[:, :, 0, col]; s = table[:, :, 1, col]
    glr = GG[:, :, 0, 127]; gli = GG[:, :, 1, 127]
    allg = sbf["BGG"]
    S.op("dve", lambda e: e.tensor_tensor(out=it[:, 0, :], in0=c, in1=glr, op=ALU.mult), reads=[Btable] + allg, writes=[Bit])
    S.op("dve", lambda e: e.tensor_tensor(out=it[:, 1, :], in0=s, in1=gli, op=ALU.mult), reads=[Btable] + allg, writes=[Bit])
    S.op("dve", lambda e: e.tensor_tensor(out=it[:, 2, :], in0=s, in1=glr, op=ALU.mult), reads=[Btable] + allg, writes=[Bit])
    S.op("dve", lambda e: e.tensor_tensor(out=it[:, 3, :], in0=c, in1=gli, op=ALU.mult), reads=[Btable] + allg, writes=[Bit])
    S.op("dve", lambda e: e.tensor_tensor(out=out[:, :, 0], in0=it[:, 0, :], in1=it[:, 1, :], op=ALU.subtract), reads=[Bit], writes=[Bout])
    S.op("dve", lambda e: e.tensor_tensor(out=out[:, :, 1], in0=it[:, 2, :], in1=it[:, 3, :], op=ALU.add), reads=[Bit], writes=[Bout])


def ssm_local_tile(C, tb, sbf, u_bf, Bu, tok0, first, PSB, PSC, BPSC):
    S = C.S
    LB, BLB = tb["LB"], tb["BLB"]
    CS, SC, Bcs = tb["CS"], tb["SC"], tb["Bcs"]
    GG = sbf["GG"]
    if first:
        S.op("pool", lambda e: e.memset(sbf["init"][:], 0.0), writes=[sbf["Binit"]])
    for st in range(16):
        ct = st // 4
        pb, Bpb = PSB.next()
        for ri in range(2):
            S.op("pe", lambda e, st=st, ri=ri, pb=pb, ct=ct: e.matmul(pb[:, ri * 128:(ri + 1) * 128], lhsT=LB[:, st, ri, :], rhs=u_bf[:, ct, tok0:tok0 + 128], start=True, stop=True),
                 reads=[BLB, Bu], writes=[Bpb], same_ok=True)
        bu = pb[:, 0:256].rearrange("p (a b) -> p a b", a=2)
        rt, Brt = sbf["rt"].next()
        S.op("dve", lambda e, st=st, rt=rt, bu=bu: e.tensor_tensor(out=rt[:, 0], in0=bu, in1=CS[:, st], op=ALU.mult), reads=[Bpb, Bcs], writes=[Brt])
        S.op("dve", lambda e, st=st, rt=rt, bu=bu: e.tensor_tensor(out=rt[:, 1], in0=bu, in1=SC[:, st], op=ALU.mult), reads=[Bpb, Bcs], writes=[Brt])
        bp, Bbp = sbf["bp"].next()
        S.op("dve", lambda e, rt=rt, bp=bp: e.tensor_tensor(out=bp[:, 0, :], in0=rt[:, 0, 0, :], in1=rt[:, 0, 1, :], op=ALU.add), reads=[Brt], writes=[Bbp])
        S.op("dve", lambda e, rt=rt, bp=bp: e.tensor_tensor(out=bp[:, 1, :], in0=rt[:, 1, 1, :], in1=rt[:, 1, 0, :], op=ALU.subtract), reads=[Brt], writes=[Bbp])
        for ri in range(2):
            S.op("dve", lambda e, st=st, ri=ri, bp=bp: e.tensor_tensor_scan(out=GG[:, st, ri, :], data0=tb["R"][:, st, :], data1=bp[:, ri, :], initial=sbf["init"][:, st, ri:ri + 1], op0=ALU.mult, op1=ALU.add),
                 reads=[tb["BR"], Bbp, sbf["Binit"]], writes=[sbf["BGG"][st]])
    ssm_rot_out_and_C(C, tb, sbf, range(16), PSC, BPSC)


def ssm_corr_tile(C, tb, sbf, PSC, BPSC):
    S = C.S
    GG = sbf["GG"]
    for st in range(16):
        for ri in range(2):
            S.op("dve", lambda e, st=st, ri=ri: e.tensor_scalar(out=GG[:, st, ri, :], in0=tb["Rp"][:, st, :], scalar1=sbf["init"][:, st, ri:ri + 1], scalar2=None, op0=ALU.mult),
                 reads=[tb["BR"], sbf["Binit"]], writes=[sbf["BGG"][st]])
    ssm_rot_out_and_C(C, tb, sbf, range(16), PSC, BPSC)


def load_consts(C, ph, d):
    S = C.S
    o = {}
    B = S.buf("consts")
    for nm, shape in (("ident", [128, 128]), ("ones", [128, 128]), ("taus", [128, 128]), ("taus1", [128, 128]), ("t128", [128, 1]), ("trimask", [128, 128])):
        t = sb(C, ph, shape, F32, "c_" + nm)
        S.dma("sp", lambda e, t=t, nm=nm: e.dma_start(out=t[:], in_=d[nm]), B, writes=[B])
        o[nm] = t
    o["B"] = B
    identb = sb(C, ph, [128, 128], BF16, "c_identb")
    S.op("dve", lambda e: e.tensor_copy(out=identb[:], in_=o["ident"][:]), reads=[B], writes=[B])
    o["identb"] = identb
    return o


def load_w_bf16(C, ph, w_dram, col0, ncols, name, stage, kchunks=8):
    S = C.S
    wb = sb(C, ph, [128, kchunks, ncols], BF16, name)
    Bw = S.buf(name)
    engs = ("pool", "act")
    n = 0
    for kc in range(kchunks):
        for c0 in range(0, ncols, 2048):
            cw = min(2048, ncols - c0)
            stg, Bst = stage.next()
            S.dma("sp", lambda e, kc=kc, c0=c0, cw=cw, stg=stg: e.dma_start(out=stg[:, :cw], in_=w_dram[kc * 128:(kc + 1) * 128, col0 + c0:col0 + c0 + cw]), Bst, writes=[Bst])
            eng = engs[n % 2]
            n += 1
            if eng == "act":
                S.op("act", lambda e, kc=kc, c0=c0, cw=cw, stg=stg: e.copy(out=wb[:, kc, c0:c0 + cw], in_=stg[:, :cw]), reads=[Bst], writes=[Bw])
            else:
                S.op("pool", lambda e, kc=kc, c0=c0, cw=cw, stg=stg: e.tensor_copy(out=wb[:, kc, c0:c0 + cw], in_=stg[:, :cw]), reads=[Bst], writes=[Bw])
    return wb, Bw


def proj_fm(C, PSr, xb, Bx, wb, Bw, col, ntok=512, kchunks=8, tok0=0):
    S = C.S
    pt, Bp = PSr.next()
    for kc in range(kchunks):
        S.op("pe", lambda e, kc=kc, pt=pt: e.matmul(pt[:, 0:ntok], lhsT=wb[:, kc, col:col + 128], rhs=xb[:, kc, tok0:tok0 + ntok], start=(kc == 0), stop=(kc == kchunks - 1)),
             reads=[Bx, Bw], writes=[Bp], same_ok=True)
    return pt, Bp


def proj_tm(C, PSr, xb, Bx, wb, Bw, col, tok0, ncols=512, kchunks=8):
    S = C.S
    pt, Bp = PSr.next()
    for kc in range(kchunks):
        S.op("pe", lambda e, kc=kc, pt=pt: e.matmul(pt[:, 0:ncols], lhsT=xb[:, kc, tok0:tok0 + 128], rhs=wb[:, kc, col:col + ncols], start=(kc == 0), stop=(kc == kchunks - 1)),
             reads=[Bx, Bw], writes=[Bp], same_ok=True)
    return pt, Bp


def layer_norm_tm(C, eng_pool, x, Bx, out, Bout, g_rep, b_rep, Bgb, stat, Bstat, width):
    S = C.S
    nch = (width + 511) // 512
    st6, mv, rstd = stat
    for c in range(nch):
        S.op("dve", lambda e, c=c: e.bn_stats(out=st6[:, c, :], in_=x[:, c * 512:min(width, (c + 1) * 512)]), reads=[Bx], writes=[Bstat])
    S.op("dve", lambda e: e.bn_aggr(out=mv[:], in_=st6[:, 0:nch, :]), reads=[Bstat], writes=[Bstat])
    S.op("act", lambda e: e.activation(out=rstd[:], in_=mv[:, 1:2], func=AF.Sqrt, bias=C.eps[:, 0:1], scale=1.0), reads=[Bstat], writes=[Bstat])
    S.op("dve", lambda e: e.reciprocal(out=rstd[:], in_=rstd[:]), reads=[Bstat], writes=[Bstat])
    S.op("dve", lambda e: e.tensor_scalar(out=x, in0=x, scalar1=mv[:, 0:1], scalar2=rstd[:, 0:1], op0=ALU.subtract, op1=ALU.mult), reads=[Bx, Bstat], writes=[Bx])
    S.op(eng_pool, lambda e: e.tensor_tensor(out=x, in0=x, in1=g_rep, op=ALU.mult), reads=[Bx, Bgb], writes=[Bx])
    S.op(eng_pool, lambda e: e.tensor_tensor(out=out, in0=x, in1=b_rep, op=ALU.add), reads=[Bx, Bgb], writes=[Bout])


def load_xblock(C, d, i, xs, Bxs, xb, Bxb):
    S = C.S
    for h in range(2):
        S.dma("sp", lambda e, i=i, h=h: e.dma_start(out=xs[:], in_=d["xT"][h * 512:(h + 1) * 512, i * 512:(i + 1) * 512].rearrange("(k p) m -> p k m", p=128)), Bxs, reads=[C.BoutT_dram], writes=[Bxs])
        S.op("pool", lambda e, h=h: e.tensor_copy(out=xb[:, h * 4:(h + 1) * 4, :], in_=xs[:]), reads=[Bxs], writes=[Bxb])


def phase_A_sgu(C, d, consts, PS):
    S = C.S
    with ExitStack() as ph:
        stage = Rot([(sb(C, ph, [128, 2048], F32, f"wst{i}"), S.buf(f"wst{i}")) for i in range(2)])
        wb, Bw = load_w_bf16(C, ph, d["w_in"], 0, 1024, "wA1", stage)
        Bsm = S.buf("smallA1")
        g_rep = sb(C, ph, [128, 512], F32, "sgug"); b_rep = sb(C, ph, [128, 512], F32, "sgub")
        wsT = sb(C, ph, [128, 4, 128], F32, "wsT"); bs_rep = sb(C, ph, [128, 4, 128], F32, "bsrep")
        for t, nm in ((g_rep, "sgu_g_rep"), (b_rep, "sgu_b_rep"), (wsT, "sgu_wT"), (bs_rep, "sgu_bs_rep")):
            S.dma("sp", lambda e, t=t, nm=nm: e.dma_start(out=t[:], in_=d[nm]), Bsm, writes=[Bsm])
        wsb = sb(C, ph, [128, 4, 128], BF16, "wsb")
        Bwsb = S.buf("wsb")
        for g in range(4):
            S.op("dve", lambda e, g=g: e.tensor_tensor(out=wsb[:, g, :], in0=wsT[:, g, :], in1=consts["trimask"][:], op=ALU.mult), reads=[Bsm, consts["B"]], writes=[Bwsb])
        xs = sb(C, ph, [128, 4, 512], F32, "xs"); Bxs = S.buf("xs")
        xbr = Rot([(sb(C, ph, [128, 8, 512], BF16, f"xb{i}"), S.buf(f"xb{i}")) for i in range(2)])
        uT = sb(C, ph, [128, 4, 512], BF16, "uT"); BuT = S.buf("uT")
        vg = Rot([(sb(C, ph, [128, 512], F32, f"vg{i}"), S.buf(f"vg{i}")) for i in range(2)])
        vln = Rot([(sb(C, ph, [128, 512], BF16, f"vln{i}"), S.buf(f"vln{i}")) for i in range(2)])
        stat = (sb(C, ph, [128, 2, 6], F32, "st6"), sb(C, ph, [128, 2], F32, "mv"), sb(C, ph, [128, 1], F32, "rstd")); Bstat = S.buf("stat")
        mtmp = sb(C, ph, [128, 4, 128], F32, "mtmp"); Bmtmp = S.buf("mtmp")
        bra = Rot([(sb(C, ph, [128, 4, 512], BF16, f"bra{i}"), S.buf(f"bra{i}")) for i in range(2)])
        PSA = Rot(PS[0:2]); PSV = Rot(PS[2:4]); PSM = Rot(PS[4:6])
        for i in range(NBLK):
            xb, Bxb = xbr.next()
            load_xblock(C, d, i, xs, Bxs, xb, Bxb)
            for g in range(4):
                pt, Bp = proj_fm(C, PSA, xb, Bxb, wb, Bw, g * 128)
                S.op("act", lambda e, g=g, pt=pt: e.activation(out=uT[:, g, :], in_=pt[:], func=AF.Gelu_apprx_tanh), reads=[Bp], writes=[BuT])
            brat, Bbra = bra.next()
            for c4 in range(4):
                pt, Bp = proj_tm(C, PSV, xb, Bxb, wb, Bw, 512, c4 * 128)
                vgt, Bvg = vg.next()
                S.op("act", lambda e, pt=pt, vgt=vgt: e.activation(out=vgt[:], in_=pt[:], func=AF.Gelu_apprx_tanh), reads=[Bp], writes=[Bvg])
                vl, Bvl = vln.next()
                layer_norm_tm(C, "pool", vgt[:], Bvg, vl[:], Bvl, g_rep[:], b_rep[:], Bsm, stat, Bstat, 512)
                pm, Bpm = PSM.next()
                for g in range(4):
                    S.op("pe", lambda e, g=g, vl=vl, pm=pm: e.matmul(pm[:, g * 128:(g + 1) * 128], lhsT=vl[:, g * 128:(g + 1) * 128], rhs=wsb[:, g, :], start=True, stop=True),
                         reads=[Bvl, Bwsb], writes=[Bpm], same_ok=True)
                S.op("dve", lambda e, pm=pm: e.tensor_tensor(out=mtmp[:].rearrange("p a b -> p (a b)"), in0=pm[:], in1=bs_rep[:].rearrange("p a b -> p (a b)"), op=ALU.add), reads=[Bpm, Bsm], writes=[Bmtmp])
                S.op("pool", lambda e, c4=c4, brat=brat: e.tensor_tensor(out=brat[:, :, c4 * 128:(c4 + 1) * 128], in0=mtmp[:], in1=uT[:, :, c4 * 128:(c4 + 1) * 128], op=ALU.mult), reads=[Bmtmp, BuT], writes=[Bbra])
            S.dma("sp", lambda e, i=i, brat=brat: e.dma_start(out=d["braT"][:, i * 512:(i + 1) * 512].rearrange("(g p) m -> p g m", p=128), in_=brat[:]), Bbra, reads=[Bbra])
        C.outbufs += [it[1] for it in bra.items]
        S.barrier()


def phase_A_ssm(C, d, consts, PS):
    S = C.S
    with ExitStack() as ph:
        tb = ssm_tables(C, ph, d, consts, PS)
        sbf = ssm_state_bufs(C, ph)
        stage = Rot([(sb(C, ph, [128, 2048], F32, f"wst{i}"), S.buf(f"wst{i}")) for i in range(2)])
        wb, Bw = load_w_bf16(C, ph, d["w_in"], 1024, 512, "wA2", stage)
        Bsm = S.buf("smallA2")
        dcol = sb(C, ph, [128, 4], F32, "dcol")
        S.dma("sp", lambda e: e.dma_start(out=dcol[:], in_=d["d_t"]), Bsm, writes=[Bsm])
        xs = sb(C, ph, [128, 4, 512], F32, "xs"); Bxs = S.buf("xs")
        xbr = Rot([(sb(C, ph, [128, 8, 512], BF16, f"xb{i}"), S.buf(f"xb{i}")) for i in range(1)])
        ubf = Rot([(sb(C, ph, [128, 4, 512], BF16, f"ubf{i}"), S.buf(f"ubf{i}")) for i in range(2)])
        uf = Rot([(sb(C, ph, [128, 4, 512], F32, f"uf{i}"), S.buf(f"uf{i}")) for i in range(1)])
        yl = Rot([(sb(C, ph, [128, 4, 512], F32, f"yl{i}"), S.buf(f"yl{i}")) for i in range(1)])
        aend = Rot([(sb(C, ph, [128, 16, 2], F32, f"aend{i}"), S.buf(f"aend{i}")) for i in range(2)])
        PSA = Rot(PS[0:2]); PSB = Rot(PS[2:6]); PSC, BPSC = PS[7]
        for i in range(NBLK):
            xb, Bxb = xbr.next()
            load_xblock(C, d, i, xs, Bxs, xb, Bxb)
            ub, Bub = ubf.next(); uff, Buf_ = uf.next()
            for g in range(4):
                pt, Bp = proj_fm(C, PSA, xb, Bxb, wb, Bw, g * 128)
                S.op("act", lambda e, g=g, pt=pt, ub=ub: e.copy(out=ub[:, g, :], in_=pt[:]), reads=[Bp], writes=[Bub])
                S.op("act", lambda e, g=g, pt=pt, uff=uff: e.copy(out=uff[:, g, :], in_=pt[:]), reads=[Bp], writes=[Buf_])
            ylt, Byl = yl.next()
            for c4 in range(4):
                ssm_local_tile(C, tb, sbf, ub, Bub, c4 * 128, c4 == 0, PSB, PSC, BPSC)
                for ct in range(4):
                    S.op("dve", lambda e, ct=ct, c4=c4, uff=uff, ylt=ylt: e.scalar_tensor_tensor(out=ylt[:, ct, c4 * 128:(c4 + 1) * 128], in0=uff[:, ct, c4 * 128:(c4 + 1) * 128], scalar=dcol[:, ct:ct + 1], in1=PSC[:, ct * 128:(ct + 1) * 128], op0=ALU.mult, op1=ALU.add),
                         reads=[Buf_, Bsm, BPSC], writes=[Byl])
                if c4 < 3:
                    ssm_next_init(C, tb, sbf, tb["CT"], tb["Bct"], 0, sbf["init"], sbf["Binit"])
                else:
                    ae, Bae = aend.next()
                    ssm_next_init(C, tb, sbf, tb["CS"], tb["Bcs"], 127, ae, Bae)
                    S.dma("sp", lambda e, i=i, ae=ae: e.dma_start(out=d["aend"][i], in_=ae[:]), Bae, reads=[Bae])
            S.dma("sp", lambda e, i=i, ylt=ylt: e.dma_start(out=d["ylocT"][:, i * 512:(i + 1) * 512].rearrange("(g p) m -> p g m", p=128), in_=ylt[:]), Byl, reads=[Byl])
        C.outbufs += [it[1] for it in yl.items + aend.items]
        S.barrier()


def core_token_index(c):
    r = c % 4
    idx = np.concatenate([np.arange((4 * i + r) * 512, (4 * i + r + 1) * 512) for i in range(8)])
    return c // 4, idx


def host_consts():
    o = {}
    o["ident"] = np.eye(128, dtype=np.float32)
    o["ones"] = np.ones((128, 128), np.float32)
    o["taus"] = np.tile(np.arange(128, dtype=np.float32), (128, 1))
    o["taus1"] = o["taus"] + 1.0
    o["t128"] = np.full((128, 1), 128.0, np.float32)
    s = np.arange(128)
    o["trimask"] = (s[:, None] <= s[None, :]).astype(np.float32)
    return o


def host_layer_params(inp, l):
    f = np.float32
    o = {}
    o["w_in"] = np.ascontiguousarray(inp["w_in"][l])
    o["sgu_g_rep"] = np.ascontiguousarray(np.tile(inp["sgu_ln_g"][l][None, :], (128, 1)))
    o["sgu_b_rep"] = np.ascontiguousarray(np.tile(inp["sgu_ln_b"][l][None, :], (128, 1)))
    o["sgu_wT"] = np.ascontiguousarray(inp["sgu_w"][l].transpose(2, 0, 1))
    o["sgu_bs_rep"] = np.ascontiguousarray(np.tile(inp["sgu_b"][l][None, :, :], (128, 1, 1)))

    def st_layout(a):
        return np.ascontiguousarray(a.reshape(16, 2, 64).transpose(1, 2, 0).reshape(128, 16))
    o["lamre"] = st_layout(inp["ssm_lambda_re"][l])
    o["lamim"] = st_layout(inp["ssm_lambda_im"][l])
    o["ldt"] = st_layout(np.tile(inp["ssm_log_dt"][l][:, None], (1, 64)))
    Bt_re = np.zeros((128, 16, 128), f); Bt_im = np.zeros((128, 16, 128), f)
    Ct_re = np.zeros((128, 16, 128), f); Ct_im = np.zeros((128, 16, 128), f)
    for g in range(32):
        st, gg = g // 2, g % 2
        r0 = 16 * (g % 8)
        Bt_re[r0:r0 + 16, st, gg * 64:(gg + 1) * 64] = inp["ssm_b_re"][l, g].T
        Bt_im[r0:r0 + 16, st, gg * 64:(gg + 1) * 64] = inp["ssm_b_im"][l, g].T
        Ct_re[gg * 64:(gg + 1) * 64, st, r0:r0 + 16] = inp["ssm_c_re"][l, g].T
        Ct_im[gg * 64:(gg + 1) * 64, st, r0:r0 + 16] = inp["ssm_c_im"][l, g].T
    o.update(Bt_re=Bt_re, Bt_im=Bt_im, Ct_re=Ct_re, Ct_im=Ct_im)
    o["d_t"] = np.ascontiguousarray(inp["ssm_d"][l].reshape(4, 128).T)
    return o


DN_ALPHA = (2 * 2) ** 0.25


def phase_A_qkvg(C, d, consts, PS):
    S = C.S
    with ExitStack() as ph:
        stage = Rot([(sb(C, ph, [128, 2048], F32, f"wst{i}"), S.buf(f"wst{i}")) for i in range(2)])
        wb, Bw = load_w_bf16(C, ph, d["w_in"], 1536, 4608, "wA3", stage)
        xs = sb(C, ph, [128, 4, 512], F32, "xs"); Bxs = S.buf("xs")
        xbr = Rot([(sb(C, ph, [128, 8, 512], BF16, f"xb{i}"), S.buf(f"xb{i}")) for i in range(2)])
        qk = Rot([(sb(C, ph, [128, 8, 512], BF16, f"qk{i}"), S.buf(f"qk{i}")) for i in range(2)])
        gs = Rot([(sb(C, ph, [128, 8, 512], BF16, f"gs{i}"), S.buf(f"gs{i}")) for i in range(3)])
        vs = Rot([(sb(C, ph, [128, 4, 512], BF16, f"vs{i}"), S.buf(f"vs{i}")) for i in range(2)])
        PSA = Rot(PS[0:4]); PSV = Rot(PS[4:6])
        for i in range(NBLK):
            xb, Bxb = xbr.next()
            load_xblock(C, d, i, xs, Bxs, xb, Bxb)
            qkt, Bqk = qk.next()
            for g in range(8):
                pt, Bp = proj_fm(C, PSA, xb, Bxb, wb, Bw, g * 128)
                if g % 2 == 0:
                    S.op("dve", lambda e, g=g, pt=pt, qkt=qkt: e.tensor_copy(out=qkt[:, g, :], in_=pt[:]), reads=[Bp], writes=[Bqk])
                else:
                    S.op("act", lambda e, g=g, pt=pt, qkt=qkt: e.copy(out=qkt[:, g, :], in_=pt[:]), reads=[Bp], writes=[Bqk])
            S.dma("sp", lambda e, i=i, qkt=qkt: e.dma_start(out=d["qT"][:, i * 512:(i + 1) * 512].rearrange("(g p) m -> p g m", p=128), in_=qkt[:, 0:4, :]), Bqk, reads=[Bqk])
            S.dma("sp", lambda e, i=i, qkt=qkt: e.dma_start(out=d["kT"][:, i * 512:(i + 1) * 512].rearrange("(g p) m -> p g m", p=128), in_=qkt[:, 4:8, :]), Bqk, reads=[Bqk])
            vst, Bvs = vs.next()
            for c4 in range(4):
                pt, Bp = proj_tm(C, PSV, xb, Bxb, wb, Bw, 1024, c4 * 128)
                S.op("dve", lambda e, c4=c4, pt=pt, vst=vst: e.tensor_copy(out=vst[:, c4, :], in_=pt[:]), reads=[Bp], writes=[Bvs])
            S.dma("sp", lambda e, i=i, vst=vst: e.dma_start(out=d["v"][i * 512:(i + 1) * 512, :].rearrange("(c p) m -> p c m", p=128), in_=vst[:]), Bvs, reads=[Bvs])
            for g3 in range(3):
                gst, Bgs = gs.next()
                for g in range(8):
                    pt, Bp = proj_fm(C, PSA, xb, Bxb, wb, Bw, 1536 + (g3 * 8 + g) * 128)
                    S.op("act", lambda e, g=g, pt=pt, gst=gst: e.activation(out=gst[:, g, :], in_=pt[:], func=AF.Sigmoid), reads=[Bp], writes=[Bgs])
                S.dma("sp", lambda e, i=i, g3=g3, gst=gst: e.dma_start(out=d["gatesT"][g3 * 1024:(g3 + 1) * 1024, i * 512:(i + 1) * 512].rearrange("(g p) m -> p g m", p=128), in_=gst[:]), Bgs, reads=[Bgs])
        C.outbufs += [it[1] for it in qk.items + gs.items + vs.items]
        S.barrier()


def ln_residual_tm(C, ps_halves, Bps, xres, Bxres, out, Bout, g_rep, b_rep, Bgb, tmp, Btmp, stat, Bstat):
    S = C.S
    for h in range(2):
        S.op("dve", lambda e, h=h: e.scalar_tensor_tensor(out=tmp[:, h * 512:(h + 1) * 512], in0=xres[:, h * 512:(h + 1) * 512], scalar=DN_ALPHA, in1=ps_halves[h], op0=ALU.mult, op1=ALU.add),
             reads=[Bxres, Bps[h]], writes=[Btmp])
    layer_norm_tm(C, "pool", tmp, Btmp, out, Bout, g_rep, b_rep, Bgb, stat, Bstat, 1024)


def phase_C1(C, d, consts, PS, l):
    S = C.S
    with ExitStack() as ph:
        stage = Rot([(sb(C, ph, [128, 2048], F32, f"wst{i}"), S.buf(f"wst{i}")) for i in range(2)])
        wbr = []
        for nm in ("w_a", "w_b", "w_c"):
            wbr.append(load_w_bf16(C, ph, d[nm], 0, 1024, nm + "b", stage, kchunks=4))
        wo, Bwo = load_w_bf16(C, ph, d["w_out"], 0, 1024, "wob", stage)
        g_rep = sb(C, ph, [128, 1024], F32, "ln1g"); b_rep = sb(C, ph, [128, 1024], F32, "ln1b"); Bgb = S.buf("ln1gb")
        S.dma("sp", lambda e: e.dma_start(out=g_rep[:], in_=d["ln1_g_rep"]), Bgb, writes=[Bgb])
        S.dma("sp", lambda e: e.dma_start(out=b_rep[:], in_=d["ln1_b_rep"]), Bgb, writes=[Bgb])
        brs = [Rot([(sb(C, ph, [128, 4, 512], BF16, f"br{k}_{i}"), S.buf(f"br{k}_{i}")) for i in range(2)]) for k in range(3)]
        gt = Rot([(sb(C, ph, [128, 24, 512], BF16, f"gt{i}"), S.buf(f"gt{i}")) for i in range(2)])
        xt = Rot([(sb(C, ph, [128, 4, 1024], F32, f"xt{i}"), S.buf(f"xt{i}")) for i in range(1)])
        mT = sb(C, ph, [128, 8, 512], BF16, "mT"); BmT = S.buf("mT")
        macc = Rot([(sb(C, ph, [128, 512], F32, f"macc{i}"), S.buf(f"macc{i}")) for i in range(2)])
        mt2 = Rot([(sb(C, ph, [128, 512], F32, f"mt2{i}"), S.buf(f"mt2{i}")) for i in range(2)])
        tmp = Rot([(sb(C, ph, [128, 1024], F32, f"lt{i}"), S.buf(f"lt{i}")) for i in range(2)])
        x1 = Rot([(sb(C, ph, [128, 1024], F32, f"x1{i}"), S.buf(f"x1{i}")) for i in range(2)])
        x1T = Rot([(sb(C, ph, [128, 8, 512], BF16, f"x1T{i}"), S.buf(f"x1T{i}")) for i in range(2)])
        stat = (sb(C, ph, [128, 2, 6], F32, "st6"), sb(C, ph, [128, 2], F32, "mv"), sb(C, ph, [128, 1], F32, "rstd")); Bstat = S.buf("stat")
        PSA = Rot(PS[0:3]); PSO = Rot(PS[3:7]); PST = Rot(PS[7:8])
        srcs = ("braT", "brbT", "brcT")
        for i in range(NBLK):
            brt = []
            for k in range(3):
                t, B = brs[k].next()
                S.dma("sp", lambda e, i=i, k=k, t=t: e.dma_start(out=t[:], in_=d[srcs[k]][:, i * 512:(i + 1) * 512].rearrange("(g p) m -> p g m", p=128)), B, reads=([C.Bbrb_dram] if k == 1 else []), writes=[B])
                brt.append((t, B))
            g_, Bg = gt.next()
            S.dma("sp", lambda e, i=i, g_=g_: e.dma_start(out=g_[:], in_=d["gatesT"][:, i * 512:(i + 1) * 512].rearrange("(g p) m -> p g m", p=128)), Bg, writes=[Bg])
            x_, Bx = xt.next()
            S.dma("sp", lambda e, i=i, x_=x_: e.dma_start(out=x_[:], in_=d["x_tok"][i * 512:(i + 1) * 512, :].rearrange("(c p) m -> p c m", p=128)), Bx, writes=[Bx])
            for dt_ in range(8):
                ma, Bma = macc.next()
                for k in range(3):
                    pt, Bp = proj_fm(C, PSA, brt[k][0], brt[k][1], wbr[k][0], wbr[k][1], dt_ * 128, kchunks=4)
                    if k == 0:
                        S.op("dve", lambda e, pt=pt, ma=ma, g_=g_, k=k, dt_=dt_: e.tensor_tensor(out=ma[:], in0=pt[:], in1=g_[:, k * 8 + dt_, :], op=ALU.mult), reads=[Bp, Bg], writes=[Bma])
                    else:
                        m2, Bm2 = mt2.next()
                        S.op("dve", lambda e, pt=pt, m2=m2, g_=g_, k=k, dt_=dt_: e.tensor_tensor(out=m2[:], in0=pt[:], in1=g_[:, k * 8 + dt_, :], op=ALU.mult), reads=[Bp, Bg], writes=[Bm2])
                        if k == 1:
                            S.op("pool", lambda e, ma=ma, m2=m2: e.tensor_tensor(out=ma[:], in0=ma[:], in1=m2[:], op=ALU.add), reads=[Bma, Bm2], writes=[Bma])
                        else:
                            S.op("pool", lambda e, ma=ma, m2=m2, dt_=dt_: e.tensor_tensor(out=mT[:, dt_, :], in0=ma[:], in1=m2[:], op=ALU.add), reads=[Bma, Bm2], writes=[BmT])
            x1Tt, Bx1T = x1T.next()
            for c4 in range(4):
                pss = []
                for h in range(2):
                    pt, Bp = proj_tm(C, PSO, mT, BmT, wo, Bwo, h * 512, c4 * 128)
                    pss.append((pt, Bp))
                tm, Btm = tmp.next()
                x1t, Bx1 = x1.next()
                ln_residual_tm(C, [pss[0][0][:], pss[1][0][:]], [pss[0][1], pss[1][1]], x_[:, c4, :], Bx, x1t[:], Bx1, g_rep[:], b_rep[:], Bgb, tm[:], Btm, stat, Bstat)
                S.dma("sp", lambda e, i=i, c4=c4, x1t=x1t: e.dma_start(out=d["x1_tok"][i * 512 + c4 * 128:i * 512 + (c4 + 1) * 128, :], in_=x1t[:]), Bx1, reads=[Bx1], writes=[C.Bx1_dram])
                for h in range(2):
                    pt, Bp = PST.next()
                    for k in range(4):
                        S.op("pe", lambda e, h=h, k=k, pt=pt, x1t=x1t: e.transpose(pt[:, k * 128:(k + 1) * 128], x1t[:, (h * 4 + k) * 128:(h * 4 + k + 1) * 128], consts["ident"][:]),
                             reads=[Bx1, consts["B"]], writes=[Bp], same_ok=True)
                    S.op("act", lambda e, h=h, c4=c4, pt=pt, x1Tt=x1Tt: e.copy(out=x1Tt[:, h * 4:(h + 1) * 4, c4 * 128:(c4 + 1) * 128], in_=pt[:].rearrange("p (a b) -> p a b", a=4)), reads=[Bp], writes=[Bx1T])
            S.dma("sp", lambda e, i=i, x1Tt=x1Tt: e.dma_start(out=d["x1T"][:, i * 512:(i + 1) * 512].rearrange("(g p) m -> p g m", p=128), in_=x1Tt[:]), Bx1T, reads=[Bx1T], writes=[C.Bx1T_dram])
        C.outbufs += [it[1] for it in x1.items + x1T.items]
        S.barrier()


def phase_C2(C, d, consts, PS, l, last):
    S = C.S
    with ExitStack() as ph:
        stage = Rot([(sb(C, ph, [128, 1024], F32, f"wst{i}"), S.buf(f"wst{i}")) for i in range(2)])
        w1, Bw1 = load_w_bf16_small(C, ph, d["ffn_w1"], 2816, "w1b", stage, 8)
        w3, Bw3 = load_w_bf16_small(C, ph, d["ffn_w3"], 2816, "w3b", stage, 8)
        w2, Bw2 = load_w_bf16_small(C, ph, d["ffn_w2"], 1024, "w2b", stage, 22)
        g_rep = sb(C, ph, [128, 1024], F32, "ln2g"); b_rep = sb(C, ph, [128, 1024], F32, "ln2b"); Bgb = S.buf("ln2gb")
        S.dma("sp", lambda e: e.dma_start(out=g_rep[:], in_=d["ln2_g_rep"]), Bgb, writes=[Bgb])
        S.dma("sp", lambda e: e.dma_start(out=b_rep[:], in_=d["ln2_b_rep"]), Bgb, writes=[Bgb])
        xT = Rot([(sb(C, ph, [128, 8, 512], BF16, f"fxT{i}"), S.buf(f"fxT{i}")) for i in range(1)])
        hT = sb(C, ph, [128, 22, 512], BF16, "hT"); BhT = [S.buf(f"hT{f}") for f in range(22)]
        sl = Rot([(sb(C, ph, [128, 512], F32, f"sl{i}"), S.buf(f"sl{i}")) for i in range(2)])
        xr = Rot([(sb(C, ph, [128, 1024], F32, f"xr{i}"), S.buf(f"xr{i}")) for i in range(1)])
        tmp = Rot([(sb(C, ph, [128, 1024], F32, f"lt{i}"), S.buf(f"lt{i}")) for i in range(1)])
        x2 = Rot([(sb(C, ph, [128, 1024], F32, f"x2{i}"), S.buf(f"x2{i}")) for i in range(2)])
        x2T = Rot([(sb(C, ph, [128, 4, 128], F32, f"x2T{i}"), S.buf(f"x2T{i}")) for i in range(2)])
        stat = (sb(C, ph, [128, 2, 6], F32, "st6"), sb(C, ph, [128, 2], F32, "mv"), sb(C, ph, [128, 1], F32, "rstd")); Bstat = S.buf("stat")
        PS1 = Rot(PS[0:2]); PS3 = Rot(PS[2:4]); PSO = Rot(PS[4:7]); PST = Rot(PS[7:8])
        for i in range(NBLK):
            xTt, BxT = xT.next()
            S.dma("sp", lambda e, i=i, xTt=xTt: e.dma_start(out=xTt[:], in_=d["x1T"][:, i * 512:(i + 1) * 512].rearrange("(g p) m -> p g m", p=128)), BxT, reads=[C.Bx1T_dram], writes=[BxT])
            for f in range(22):
                p1, Bp1 = proj_fm(C, PS1, xTt, BxT, w1, Bw1, f * 128)
                p3, Bp3 = proj_fm(C, PS3, xTt, BxT, w3, Bw3, f * 128)
                s_, Bs_ = sl.next()
                S.op("act", lambda e, p1=p1, s_=s_: e.activation(out=s_[:], in_=p1[:], func=AF.Silu), reads=[Bp1], writes=[Bs_])
                S.op("dve", lambda e, p3=p3, s_=s_, f=f: e.tensor_tensor(out=hT[:, f, :], in0=p3[:], in1=s_[:], op=ALU.mult), reads=[Bp3, Bs_], writes=[BhT[f]])
            for c4 in range(4):
                xr_, Bxr = xr.next()
                S.dma("sp", lambda e, i=i, c4=c4, xr_=xr_: e.dma_start(out=xr_[:], in_=d["x1_tok"][i * 512 + c4 * 128:i * 512 + (c4 + 1) * 128, :]), Bxr, reads=[C.Bx1_dram], writes=[Bxr])
                pss = []
                for h in range(2):
                    pt, Bp = PSO.next()
                    for f in range(22):
                        S.op("pe", lambda e, f=f, h=h, c4=c4, pt=pt: e.matmul(pt[:], lhsT=hT[:, f, c4 * 128:(c4 + 1) * 128], rhs=w2[:, f, h * 512:(h + 1) * 512], start=(f == 0), stop=(f == 21)),
                             reads=[BhT[f], Bw2], writes=[Bp], same_ok=True)
                    pss.append((pt, Bp))
                tm, Btm = tmp.next()
                x2t, Bx2 = x2.next()
                ln_residual_tm(C, [pss[0][0][:], pss[1][0][:]], [pss[0][1], pss[1][1]], xr_[:], Bxr, x2t[:], Bx2, g_rep[:], b_rep[:], Bgb, tm[:], Btm, stat, Bstat)
                S.dma("sp", lambda e, i=i, c4=c4, x2t=x2t: e.dma_start(out=d["out_tok"][i * 512 + c4 * 128:i * 512 + (c4 + 1) * 128, :], in_=x2t[:]), Bx2, reads=[Bx2], writes=[C.Bout_dram])
                if not last:
                    for h in range(2):
                        pt, Bp = PST.next()
                        for k in range(4):
                            S.op("pe", lambda e, h=h, k=k, pt=pt, x2t=x2t: e.transpose(pt[:, k * 128:(k + 1) * 128], x2t[:, (h * 4 + k) * 128:(h * 4 + k + 1) * 128], consts["ident"][:]),
                                 reads=[Bx2, consts["B"]], writes=[Bp], same_ok=True)
                        xo, Bxo = x2T.next()
                        S.op("act", lambda e, pt=pt, xo=xo: e.copy(out=xo[:].rearrange("p a b -> p (a b)"), in_=pt[:]), reads=[Bp], writes=[Bxo])
                        S.dma("sp", lambda e, i=i, c4=c4, h=h, xo=xo: e.dma_start(out=d["outT"][h * 512:(h + 1) * 512, i * 512 + c4 * 128:i * 512 + (c4 + 1) * 128].rearrange("(g p) m -> p g m", p=128), in_=xo[:]), Bxo, reads=[Bxo], writes=[C.BoutT_dram])
        C.outbufs += [it[1] for it in x2.items + x2T.items]
        S.barrier()


def load_w_bf16_small(C, ph, w_dram, ncols, name, stage, kchunks):
    S = C.S
    wb = sb(C, ph, [128, kchunks, ncols], BF16, name)
    Bw = S.buf(name)
    n = 0
    for kc in range(kchunks):
        for c0 in range(0, ncols, 1024):
            cw = min(1024, ncols - c0)
            stg, Bst = stage.next()
            S.dma("sp", lambda e, kc=kc, c0=c0, cw=cw, stg=stg: e.dma_start(out=stg[:, :cw], in_=w_dram[kc * 128:(kc + 1) * 128, c0:c0 + cw]), Bst, writes=[Bst])
            if n % 2:
                S.op("act", lambda e, kc=kc, c0=c0, cw=cw, stg=stg: e.copy(out=wb[:, kc, c0:c0 + cw], in_=stg[:, :cw]), reads=[Bst], writes=[Bw])
            else:
                S.op("pool", lambda e, kc=kc, c0=c0, cw=cw, stg=stg: e.tensor_copy(out=wb[:, kc, c0:c0 + cw], in_=stg[:, :cw]), reads=[Bst], writes=[Bw])
            n += 1
    return wb, Bw


def phase_B_ssm(C, d, consts, PS):
    S = C.S
    with ExitStack() as ph:
        tb = ssm_tables(C, ph, d, consts, PS)
        sbf = ssm_state_bufs(C, ph)
        th = tb["th"]; Bth = tb["Bth"]
        t512 = sb(C, ph, [128, 1], F32, "t512"); Bt5 = S.buf("t512")
        S.op("pool", lambda e: e.memset(t512[:], 512.0), writes=[Bt5])
        ang = sb(C, ph, [128, 128], F32, "ang2"); angc = sb(C, ph, [128, 128], F32, "angc2")
        ti = sb(C, ph, [128, 128], I32, "tti2"); tf = sb(C, ph, [128, 128], F32, "ttf2")
        pool = (ang, angc, ti, tf, S.buf("ang2"), S.buf("angc2"), S.buf("ttmp2"))
        C5, _, Bc5 = trig_table(C, ph, th, Bth, "r512", 16, t512, pool, Bt5)
        r512 = sb(C, ph, [128, 16], F32, "r512"); lnr = sb(C, ph, [128, 16], F32, "lnr"); Br5 = S.buf("r512")
        S.op("act", lambda e: e.activation(out=lnr[:], in_=tb["r"][:], func=AF.Ln), reads=[tb["Bs"]], writes=[Br5])
        S.op("act", lambda e: e.activation(out=r512[:], in_=lnr[:], func=AF.Exp, scale=512.0), reads=[Br5], writes=[Br5])
        L5 = sb(C, ph, [128, 2, 16], F32, "L5")
        S.op("dve", lambda e: e.tensor_tensor(out=L5[:, 0, :], in0=r512[:], in1=C5[:, :, 0, 0], op=ALU.mult), reads=[Br5, Bc5], writes=[Br5])
        S.op("dve", lambda e: e.tensor_tensor(out=L5[:, 1, :], in0=r512[:], in1=C5[:, :, 1, 0], op=ALU.mult), reads=[Br5, Bc5], writes=[Br5])
        A = sb(C, ph, [128, 35, 16, 2], F32, "Aall"); BA = S.buf("Aall")
        S.dma("sp", lambda e: e.dma_start(out=A[:], in_=d["aend_all"].rearrange("j p s c -> p j s c")), BA, writes=[BA])
        Hs = sb(C, ph, [128, 35, 16, 2], F32, "Hs"); BHs = S.buf("Hs")
        S.op("pool", lambda e: e.memset(Hs[:, 0], 0.0), writes=[BHs])
        tt = sb(C, ph, [128, 4, 16], F32, "pt4"); Btt = S.buf("pt4")
        nblk_needed = 4 * (NBLK - 1) + 3 + 1
        for j in range(1, nblk_needed):
            hr = Hs[:, j - 1, :, 0]; hi = Hs[:, j - 1, :, 1]
            S.op("dve", lambda e, hr=hr: e.tensor_tensor(out=tt[:, 0, :], in0=L5[:, 0, :], in1=hr, op=ALU.mult), reads=[Br5, BHs], writes=[Btt])
            S.op("dve", lambda e, hi=hi: e.tensor_tensor(out=tt[:, 1, :], in0=L5[:, 1, :], in1=hi, op=ALU.mult), reads=[Br5, BHs], writes=[Btt])
            S.op("dve", lambda e, hr=hr: e.tensor_tensor(out=tt[:, 2, :], in0=L5[:, 1, :], in1=hr, op=ALU.mult), reads=[Br5, BHs], writes=[Btt])
            S.op("dve", lambda e, hi=hi: e.tensor_tensor(out=tt[:, 3, :], in0=L5[:, 0, :], in1=hi, op=ALU.mult), reads=[Br5, BHs], writes=[Btt])
            S.op("dve", lambda e: e.tensor_tensor(out=tt[:, 0, :], in0=tt[:, 0, :], in1=tt[:, 1, :], op=ALU.subtract), reads=[Btt], writes=[Btt])
            S.op("dve", lambda e: e.tensor_tensor(out=tt[:, 2, :], in0=tt[:, 2, :], in1=tt[:, 3, :], op=ALU.add), reads=[Btt], writes=[Btt])
            S.op("dve", lambda e, j=j: e.tensor_tensor(out=Hs[:, j, :, 0], in0=tt[:, 0, :], in1=A[:, j - 1, :, 0], op=ALU.add), reads=[Btt, BA], writes=[BHs])
            S.op("dve", lambda e, j=j: e.tensor_tensor(out=Hs[:, j, :, 1], in0=tt[:, 2, :], in1=A[:, j - 1, :, 1], op=ALU.add), reads=[Btt, BA], writes=[BHs])
        stage = Rot([(sb(C, ph, [128, 2048], F32, f"wst{i}"), S.buf(f"wst{i}")) for i in range(2)])
        gw, Bgw = load_w_bf16(C, ph, d["glu_w"], 0, 512, "gluw", stage, kchunks=4)
        gb = sb(C, ph, [128, 4], F32, "glub"); Bgb = S.buf("glub")
        S.dma("sp", lambda e: e.dma_start(out=gb[:], in_=d["glu_b_t"]), Bgb, writes=[Bgb])
        yl = Rot([(sb(C, ph, [128, 4, 512], F32, f"yl{i}"), S.buf(f"yl{i}")) for i in range(2)])
        yg = Rot([(sb(C, ph, [128, 4, 512], BF16, f"yg{i}"), S.buf(f"yg{i}")) for i in range(2)])
        sg = Rot([(sb(C, ph, [128, 512], BF16, f"sg{i}"), S.buf(f"sg{i}")) for i in range(2)])
        brb = Rot([(sb(C, ph, [128, 4, 512], BF16, f"brb{i}"), S.buf(f"brb{i}")) for i in range(2)])
        PSA = Rot(PS[0:2]); PSC, BPSC = PS[7]
        for i in range(NBLK):
            j = 4 * i + 3
            ylt, Byl = yl.next()
            S.dma("sp", lambda e, i=i, ylt=ylt: e.dma_start(out=ylt[:], in_=d["ylocT"][:, i * 512:(i + 1) * 512].rearrange("(g p) m -> p g m", p=128)), Byl, writes=[Byl])
            it, Bit = sbf["itmp"], sbf["Bitmp"]
            c1 = tb["CS"][:, :, 0, 1]; s1 = tb["CS"][:, :, 1, 1]
            hr = Hs[:, j, :, 0]; hi = Hs[:, j, :, 1]
            S.op("dve", lambda e, hr=hr: e.tensor_tensor(out=it[:, 0, :], in0=c1, in1=hr, op=ALU.mult), reads=[tb["Bcs"], BHs], writes=[Bit])
            S.op("dve", lambda e, hi=hi: e.tensor_tensor(out=it[:, 1, :], in0=s1, in1=hi, op=ALU.mult), reads=[tb["Bcs"], BHs], writes=[Bit])
            S.op("dve", lambda e, hr=hr: e.tensor_tensor(out=it[:, 2, :], in0=s1, in1=hr, op=ALU.mult), reads=[tb["Bcs"], BHs], writes=[Bit])
            S.op("dve", lambda e, hi=hi: e.tensor_tensor(out=it[:, 3, :], in0=c1, in1=hi, op=ALU.mult), reads=[tb["Bcs"], BHs], writes=[Bit])
            S.op("dve", lambda e: e.tensor_tensor(out=sbf["init"][:, :, 0], in0=it[:, 0, :], in1=it[:, 1, :], op=ALU.subtract), reads=[Bit], writes=[sbf["Binit"]])
            S.op("dve", lambda e: e.tensor_tensor(out=sbf["init"][:, :, 1], in0=it[:, 2, :], in1=it[:, 3, :], op=ALU.add), reads=[Bit], writes=[sbf["Binit"]])
            ygt, Byg = yg.next()
            for c4 in range(4):
                ssm_corr_tile(C, tb, sbf, PSC, BPSC)
                S.op("dve", lambda e, c4=c4, ylt=ylt: e.tensor_tensor(out=ylt[:, :, c4 * 128:(c4 + 1) * 128], in0=ylt[:, :, c4 * 128:(c4 + 1) * 128], in1=PSC[:].rearrange("p (a b) -> p a b", a=4), op=ALU.add), reads=[Byl, BPSC], writes=[Byl])
                if c4 < 3:
                    ssm_next_init(C, tb, sbf, tb["CT"], tb["Bct"], 0, sbf["init"], sbf["Binit"])
            S.op("act", lambda e, ylt=ylt, ygt=ygt: e.activation(out=ygt[:], in_=ylt[:], func=AF.Gelu_apprx_tanh), reads=[Byl], writes=[Byg])
            brbt, Bbrb = brb.next()
            for g in range(4):
                pt, Bp = proj_fm(C, PSA, ygt, Byg, gw, Bgw, g * 128, kchunks=4)
                sgt, Bsg = sg.next()
                S.op("act", lambda e, g=g, pt=pt, sgt=sgt: e.activation(out=sgt[:], in_=pt[:], func=AF.Sigmoid, bias=gb[:, g:g + 1], scale=1.0), reads=[Bp, Bgb], writes=[Bsg])
                S.op("pool", lambda e, g=g, sgt=sgt, ygt=ygt, brbt=brbt: e.tensor_tensor(out=brbt[:, g, :], in0=ygt[:, g, :], in1=sgt[:], op=ALU.mult), reads=[Bsg, Byg], writes=[Bbrb])
            S.dma("sp", lambda e, i=i, brbt=brbt: e.dma_start(out=d["brbT"][:, i * 512:(i + 1) * 512].rearrange("(g p) m -> p g m", p=128), in_=brbt[:]), Bbrb, reads=[Bbrb], writes=[C.Bbrb_dram])
        C.outbufs += [it_[1] for it_ in brb.items]
        S.barrier()


NJ = 32


def phase_B_attn(C, d, consts, PS):
    S = C.S
    SEQL = NJ * 512
    with ExitStack() as ph:
        T2 = sb(C, ph, [68, SEQL], BF16, "T2"); BT2 = S.buf("T2")
        QS = min(4096, SEQL)
        for q4 in range(0, SEQL, QS):
            S.dma("sp", lambda e, q4=q4: e.dma_start(out=T2[:, q4:q4 + QS], in_=d["T2"][:, q4:q4 + QS]), BT2, writes=[BT2])
        trib = sb(C, ph, [128, 128], BF16, "trib"); Btr = S.buf("trib")
        S.op("dve", lambda e: e.tensor_copy(out=trib[:], in_=consts["trimask"][:]), reads=[consts["B"]], writes=[Btr])
        kTh = sb(C, ph, [64, SEQL], BF16, "kTh"); BkT = S.buf("kTh")
        vh = sb(C, ph, [128, SEQL // 128, 65], BF16, "vh"); Bvh = S.buf("vh")
        S.op("pool", lambda e: e.memset(vh[:, :, 64:65], 1.0), writes=[Bvh])
        qTh = sb(C, ph, [64, SEQL], BF16, "qTh"); BqT = S.buf("qTh")
        q2r = Rot([(sb(C, ph, [68, 512], BF16, f"q2_{i}"), S.buf(f"q2_{i}")) for i in range(2)])
        kmf = sb(C, ph, [64, 64], F32, "kmf"); kmh = sb(C, ph, [64, 64], BF16, "kmh"); kml = sb(C, ph, [64, 64], BF16, "kml"); kmt = sb(C, ph, [64, 64], F32, "kmt"); Bkm = S.buf("km")
        gsb = Rot([(sb(C, ph, [128, 64], F32, f"gsb{i}"), S.buf(f"gsb{i}")) for i in range(2)])
        m8 = Rot([(sb(C, ph, [128, 8], F32, f"m8{i}"), S.buf(f"m8{i}")) for i in range(2)])
        selm = Rot([(sb(C, ph, [128, 64], F32, f"selm{i}"), S.buf(f"selm{i}")) for i in range(2)])
        PT = Rot([(sb(C, ph, [128, 512], BF16, f"PT{i}"), S.buf(f"PT{i}")) for i in range(4)])
        rec = Rot([(sb(C, ph, [128, 1], F32, f"rec{i}"), S.buf(f"rec{i}")) for i in range(2)])
        brc = sb(C, ph, [128, NJ * 4, 128], BF16, "brc"); Bbrc = [S.buf(f"brc{i}") for i in range(NJ)]
        oT = Rot([(sb(C, ph, [128, 512], BF16, f"oT{i}"), S.buf(f"oT{i}")) for i in range(2)])
        PG, BPG = PS[0]; PTr, BPTr = PS[0]; PSS = Rot(PS[1:4]); PO = PS[4:8]
        nkm = SEQL // 256
        S.op("pool", lambda e: e.memset(kmf[:], 0.0), writes=[Bkm])

        def gating(hh, j, q2, Bq2):
            S.dma("sp", lambda e: e.dma_start(out=q2[64:68, :], in_=d["q2c"][hh, :, j * 512:(j + 1) * 512]), Bq2, writes=[Bq2])
            for c4 in range(4):
                qb = 2 * j + c4 // 2
                qc0 = j * 512 + c4 * 128
                S.op("pe", lambda e, qc0=qc0: e.matmul(PG[:, 0:64], lhsT=qTh[:, qc0:qc0 + 128], rhs=kmh[:], start=True, stop=False), reads=[BqT, Bkm], writes=[BPG], same_ok=True)
                S.op("pe", lambda e, qc0=qc0: e.matmul(PG[:, 0:64], lhsT=qTh[:, qc0:qc0 + 128], rhs=kml[:], start=False, stop=True), reads=[BqT, Bkm], writes=[BPG], same_ok=True)
                g_, Bg_ = gsb.next()
                S.op("pool", lambda e, g_=g_: e.memset(g_[:], -1e30), writes=[Bg_])
                if qb > 0:
                    S.op("dve", lambda e, g_=g_, qb=qb: e.tensor_copy(out=g_[:, 0:qb], in_=PG[:, 0:qb]), reads=[BPG], writes=[Bg_])
                m_, Bm_ = m8.next()
                S.op("dve", lambda e, g_=g_, m_=m_: e.max(out=m_[:], in_=g_[:]), reads=[Bg_], writes=[Bm_])
                s_, Bs_ = selm.next()
                S.op("dve", lambda e, g_=g_, m_=m_, s_=s_: e.tensor_scalar(out=s_[:], in0=g_[:], scalar1=m_[:, 2:3], scalar2=-1.0, op0=ALU.is_ge, op1=ALU.add), reads=[Bg_, Bm_], writes=[Bs_])
                S.op("dve", lambda e, s_=s_, qb=qb: e.memset(s_[:, qb:qb + 1], 0.0), reads=[Bs_], writes=[Bs_])
                S.op("pe", lambda e, s_=s_: e.transpose(PTr[0:64, 128:256], s_[:], consts["ident"][:]), reads=[Bs_, consts["B"], BPG], writes=[BPTr], same_ok=True)
                S.op("act", lambda e, c4=c4: e.activation(out=q2[0:64, c4 * 128:(c4 + 1) * 128], in_=PTr[0:64, 128:256], func=AF.Copy, scale=30000.0), reads=[BPTr], writes=[Bq2])

        def qk(j, t, q2, Bq2):
            tt = t - 4 * j
            c_lo = max(tt, 0)
            q0 = j * 512 + c_lo * 128
            ncol = 512 - c_lo * 128
            ps_, Bps_ = PSS.next()
            S.op("pe", lambda e: e.matmul(ps_[:, 0:ncol], lhsT=kTh[:, t * 128:(t + 1) * 128], rhs=qTh[:, q0:q0 + ncol], start=True, stop=False), reads=[BkT, BqT], writes=[Bps_], same_ok=True)
            S.op("pe", lambda e: e.matmul(ps_[:, 0:ncol], lhsT=T2[:, t * 128:(t + 1) * 128], rhs=q2[:, c_lo * 128:512], start=False, stop=True), reads=[BT2, Bq2], writes=[Bps_], same_ok=True)
            return (ps_, Bps_, tt, c_lo, ncol)

        def exp_pv(j, t, st_):
            ps_, Bps_, tt, c_lo, ncol = st_
            p_, Bp_ = PT.next()
            S.op("act", lambda e: e.activation(out=p_[:, 0:ncol], in_=ps_[:, 0:ncol], func=AF.Exp, scale=0.125), reads=[Bps_], writes=[Bp_])
            if tt >= 0:
                S.op("pool", lambda e: e.tensor_tensor(out=p_[:, 0:128], in0=p_[:, 0:128], in1=trib[:], op=ALU.mult), reads=[Bp_, Btr], writes=[Bp_])
            for c4 in range(c_lo, 4):
                po, Bpo = PO[c4]
                S.op("pe", lambda e, c4=c4, po=po: e.matmul(po[:, 0:65], lhsT=p_[:, (c4 - c_lo) * 128:(c4 - c_lo + 1) * 128], rhs=vh[:, t, :], start=(t == 0), stop=(t == 4 * j + c4)),
                     reads=[Bp_, Bvh], writes=[Bpo], same_ok=True)

        for hh in range(2):
            S.dma("sp", lambda e, hh=hh: e.dma_start(out=kTh[:], in_=d["kT_hp"][hh * 64:(hh + 1) * 64, :]), BkT, writes=[BkT])
            for q4 in range(0, SEQL, QS):
                S.dma("sp", lambda e, hh=hh, q4=q4: e.dma_start(out=vh[:, q4 // 128:(q4 + QS) // 128, 0:64], in_=d["v_hp"][q4:q4 + QS, hh * 64:(hh + 1) * 64].rearrange("(t p) m -> p t m", p=128)), Bvh, writes=[Bvh])
            S.dma("sp", lambda e, hh=hh: e.dma_start(out=qTh[:], in_=d["qT_hp"][hh * 64:(hh + 1) * 64, :]), BqT, writes=[BqT])
            S.op("dve", lambda e: e.tensor_reduce(out=kmf[:, 0:nkm], in_=kTh[:].rearrange("p (n k) -> p n k", k=256), op=ALU.add, axis=AX.X), reads=[BkT], writes=[Bkm])
            S.op("dve", lambda e: e.tensor_scalar(out=kmf[:], in0=kmf[:], scalar1=1.0 / 256, scalar2=None, op0=ALU.mult), reads=[Bkm], writes=[Bkm])
            S.op("dve", lambda e: e.tensor_copy(out=kmh[:], in_=kmf[:]), reads=[Bkm], writes=[Bkm])
            S.op("dve", lambda e: e.tensor_copy(out=kmt[:], in_=kmh[:]), reads=[Bkm], writes=[Bkm])
            S.op("dve", lambda e: e.tensor_tensor(out=kmt[:], in0=kmf[:], in1=kmt[:], op=ALU.subtract), reads=[Bkm], writes=[Bkm])
            S.op("dve", lambda e: e.tensor_copy(out=kml[:], in_=kmt[:]), reads=[Bkm], writes=[Bkm])
            q2s = [q2r.next() for _ in range(NJ)]
            gating(hh, 0, *q2s[0])
            for j in range(NJ):
                q2, Bq2 = q2s[j]
                ntile = 4 * j + 4
                pend = [qk(j, 0, q2, Bq2), qk(j, 1, q2, Bq2)]
                if j + 1 < NJ:
                    gating(hh, j + 1, *q2s[j + 1])
                for t in range(ntile):
                    if t + 2 < ntile:
                        pend.append(qk(j, t + 2, q2, Bq2))
                    exp_pv(j, t, pend.pop(0))
                for c4 in range(4):
                    po, Bpo = PO[c4]
                    rc, Brc = rec.next()
                    S.op("dve", lambda e, po=po, rc=rc: e.reciprocal(out=rc[:], in_=po[:, 64:65]), reads=[Bpo], writes=[Brc])
                    S.op("dve", lambda e, po=po, rc=rc, j=j, c4=c4, hh=hh: e.tensor_scalar(out=brc[:, j * 4 + c4, hh * 64:(hh + 1) * 64], in0=po[:, 0:64], scalar1=rc[:, 0:1], scalar2=None, op0=ALU.mult), reads=[Bpo, Brc], writes=[Bbrc[j]])
        for j in range(NJ):
            ps_, Bps_ = PSS.next()
            for c4 in range(4):
                S.op("pe", lambda e, j=j, c4=c4, ps_=ps_: e.matmul(ps_[:, c4 * 128:(c4 + 1) * 128], lhsT=brc[:, j * 4 + c4, :], rhs=consts["identb"][:], start=True, stop=True), reads=[Bbrc[j], consts["B"]], writes=[Bps_], same_ok=True)
            o_, Bo_ = oT.next()
            S.op("act", lambda e, ps_=ps_, o_=o_: e.copy(out=o_[:], in_=ps_[:]), reads=[Bps_], writes=[Bo_])
            S.dma("sp", lambda e, j=j, o_=o_: e.dma_start(out=d["brcT_hp"][:, j * 512:(j + 1) * 512], in_=o_[:]), Bo_, reads=[Bo_])
        C.outbufs += [it_[1] for it_ in oT.items]
        S.barrier()


import ml_dtypes
from concourse.bass_utils import run_bass_kernel_spmd

NPBF = ml_dtypes.bfloat16
CONST_SHAPES = {"ident": [128, 128], "ones": [128, 128], "taus": [128, 128], "taus1": [128, 128], "t128": [128, 1], "trimask": [128, 128]}
SSM_SHAPES = {"lamre": [128, 16], "lamim": [128, 16], "ldt": [128, 16], "Bt_re": [128, 16, 128], "Bt_im": [128, 16, 128],
              "Ct_re": [128, 16, 128], "Ct_im": [128, 16, 128], "d_t": [128, 4]}
A_PARAM_SHAPES = dict({"w_in": [1024, 6144], "sgu_g_rep": [128, 512], "sgu_b_rep": [128, 512], "sgu_wT": [128, 4, 128], "sgu_bs_rep": [128, 4, 128]}, **SSM_SHAPES)
A_OUT_SHAPES = {"braT": ([512, NT], BF16), "ylocT": ([512, NT], F32), "aend": ([8, 128, 16, 2], F32), "qT": ([512, NT], BF16),
                "kT": ([512, NT], BF16), "v": ([NT, 512], BF16), "gatesT": ([3072, NT], BF16)}
C_PARAM_SHAPES = dict({"glu_w": [512, 512], "glu_b_t": [128, 4], "w_a": [512, 1024], "w_b": [512, 1024], "w_c": [512, 1024], "w_out": [1024, 1024],
                       "ln1_g_rep": [128, 1024], "ln1_b_rep": [128, 1024], "ffn_w1": [1024, 2816], "ffn_w3": [1024, 2816], "ffn_w2": [2816, 1024],
                       "ln2_g_rep": [128, 1024], "ln2_b_rep": [128, 1024]}, **SSM_SHAPES)


def _setup(nc, st):
    C = Ctx(nc, st)
    C.outbufs = []
    S = C.S
    C.eps = sb(C, st, [128, 1], F32, "eps")
    S.op("pool", lambda e: e.memset(C.eps[:], 1e-5), writes=[S.buf("eps")])
    for nm in ("BoutT_dram", "Bbrb_dram", "Bx1_dram", "Bx1T_dram", "Bout_dram"):
        setattr(C, nm, Buf(nm, multi=True))
    d = {}
    for k, shp in CONST_SHAPES.items():
        d[k] = C.dram_in(k, shp)
    cs = load_consts(C, st, d)
    PS = [(C.ps([128, 512]), S.buf(f"ps{i}")) for i in range(8)]
    return C, d, cs, PS


def build_A():
    nc = bass.Bass("TRN2", target_bir_lowering=False)
    with ExitStack() as st:
        C, d, cs, PS = _setup(nc, st)
        for k, shp in A_PARAM_SHAPES.items():
            d[k] = C.dram_in(k, shp)
        d["xT"] = C.dram_in("xT", [1024, NT])
        for k, (shp, dt_) in A_OUT_SHAPES.items():
            d[k] = C.dram_out(k, shp, dt_)
        phase_A_sgu(C, d, cs, PS)
        phase_A_ssm(C, d, cs, PS)
        phase_A_qkvg(C, d, cs, PS)
        C.S.finish("sp", C.outbufs)
        C.S.emit()
    return nc


def build_B():
    nc = bass.Bass("TRN2", target_bir_lowering=False)
    SEQL = NJ * 512
    with ExitStack() as st:
        C, d, cs, PS = _setup(nc, st)
        d["T2"] = C.dram_in("T2", [68, SEQL], BF16)
        d["q2c"] = C.dram_in("q2c", [2, 4, SEQL], BF16)
        d["qT_hp"] = C.dram_in("qT_hp", [128, SEQL], BF16)
        d["kT_hp"] = C.dram_in("kT_hp", [128, SEQL], BF16)
        d["v_hp"] = C.dram_in("v_hp", [SEQL, 128], BF16)
        d["brcT_hp"] = C.dram_out("brcT_hp", [128, SEQL], BF16)
        phase_B_attn(C, d, cs, PS)
        C.S.finish("sp", C.outbufs)
        C.S.emit()
    return nc


def build_C(last):
    nc = bass.Bass("TRN2", target_bir_lowering=False)
    with ExitStack() as st:
        C, d, cs, PS = _setup(nc, st)
        for k, shp in C_PARAM_SHAPES.items():
            d[k] = C.dram_in(k, shp)
        d["aend_all"] = C.dram_in("aend_all", [35, 128, 16, 2])
        d["ylocT"] = C.dram_in("ylocT", [512, NT])
        d["braT"] = C.dram_in("braT", [512, NT], BF16)
        d["brcT"] = C.dram_in("brcT", [512, NT], BF16)
        d["gatesT"] = C.dram_in("gatesT", [3072, NT], BF16)
        d["x_tok"] = C.dram_in("x_tok", [NT, 1024])
        d["brbT"] = C.dram_out("brbT", [512, NT], BF16)
        d["x1_tok"] = C.dram_out("x1_tok", [NT, 1024])
        d["x1T"] = C.dram_out("x1T", [1024, NT], BF16)
        d["out_tok"] = C.dram_out("out_tok", [NT, 1024])
        if not last:
            d["outT"] = C.dram_out("outT", [1024, NT])
        phase_B_ssm(C, d, cs, PS)
        phase_C1(C, d, cs, PS, 0)
        phase_C2(C, d, cs, PS, 0, last)
        if not last:
            dA = dict(d)
            for k, shp in A_PARAM_SHAPES.items():
                dA[k] = C.dram_in("n_" + k, shp)
            dA["xT"] = d["outT"]
            for k, (shp, dt_) in A_OUT_SHAPES.items():
                dA[k] = C.dram_out("n_" + k, shp, dt_)
            phase_A_sgu(C, dA, cs, PS)
            phase_A_ssm(C, dA, cs, PS)
            phase_A_qkvg(C, dA, cs, PS)
        C.S.finish("sp", C.outbufs + [C.Bout_dram, C.Bbrb_dram, C.Bx1_dram, C.Bx1T_dram, C.BoutT_dram])
        C.S.emit()
    return nc


def host_c_params(inp, l):
    o = {k: v for k, v in host_layer_params(inp, l).items() if k in SSM_SHAPES}
    o["glu_w"] = np.ascontiguousarray(inp["glu_w"][l])
    o["glu_b_t"] = np.ascontiguousarray(inp["glu_b"][l].reshape(4, 128).T)
    o["w_a"] = np.ascontiguousarray(inp["w_branch_a"][l]); o["w_b"] = np.ascontiguousarray(inp["w_branch_b"][l]); o["w_c"] = np.ascontiguousarray(inp["w_branch_c"][l])
    o["w_out"] = np.ascontiguousarray(inp["w_out"][l])
    for nm in ("ln1_g", "ln1_b", "ln2_g", "ln2_b"):
        o[nm + "_rep"] = np.ascontiguousarray(np.tile(inp[nm][l][None, :], (128, 1)))
    o["ffn_w1"] = np.ascontiguousarray(inp["ffn_w1"][l]); o["ffn_w3"] = np.ascontiguousarray(inp["ffn_w3"][l]); o["ffn_w2"] = np.ascontiguousarray(inp["ffn_w2"][l])
    return o


def host_attn_consts():
    SEQL = NJ * 512
    key = np.arange(SEQL)
    T2 = np.zeros((68, SEQL), np.float32)
    T2[key // 256, key] = 1.0
    T2[64] = key % 128; T2[65] = key // 128; T2[66] = 1.0; T2[67] = 1.0
    q2 = []
    for h in range(8):
        s8 = 8.0 * 2.0 ** (-(h + 1))
        q2.append(np.stack([np.full(SEQL, s8), np.full(SEQL, s8 * 128), -s8 * (key % 128), -s8 * 128 * (key // 128)]).astype(np.float32))
    return T2.astype(NPBF), np.stack(q2).astype(NPBF)


_PROGS = {}


def _prog(name, fn):
    if name not in _PROGS:
        _PROGS[name] = fn()
    return _PROGS[name]


def kernel(**inp):
    inp = {k: np.asarray(v) for k, v in inp.items()}
    x = inp["x"]
    consts = host_consts()
    T2, q2c_all = host_attn_consts()
    cores = list(range(8))
    tok = [core_token_index(c) for c in cores]
    x_tok = [np.ascontiguousarray(x[b][idx]) for b, idx in tok]

    def exchange_for_B(resA):
        maps = []
        full = {}
        for b in range(2):
            qT = np.zeros((512, 16384), NPBF); kT = np.zeros((512, 16384), NPBF); v = np.zeros((16384, 512), NPBF)
            for r in range(4):
                c = b * 4 + r
                idx = tok[c][1]
                qT[:, idx] = resA[c]["qT"]; kT[:, idx] = resA[c]["kT"]; v[idx, :] = resA[c]["v"]
            full[b] = (qT, kT, v)
        for c in cores:
            b, hp = c // 4, c % 4
            qT, kT, v = full[b]
            m = dict(consts)
            m["T2"] = T2
            m["q2c"] = np.ascontiguousarray(q2c_all[2 * hp:2 * hp + 2])
            m["qT_hp"] = np.ascontiguousarray(qT[hp * 128:(hp + 1) * 128]); m["kT_hp"] = np.ascontiguousarray(kT[hp * 128:(hp + 1) * 128])
            m["v_hp"] = np.ascontiguousarray(v[:, hp * 128:(hp + 1) * 128])
            maps.append(m)
        return maps

    def maps_for_C(l, resA, resB, xtoks, last):
        cp = host_c_params(inp, l)
        nextp = None if last else host_layer_params(inp, l + 1)
        maps = []
        for c in cores:
            b, r = c // 4, c % 4
            idx = tok[c][1]
            m = dict(consts); m.update(cp)
            A = np.zeros((35, 128, 16, 2), np.float32)
            for j in range(32):
                A[3 - r + j] = resA[b * 4 + j % 4]["aend"][j // 4]
            m["aend_all"] = A
            m["ylocT"] = resA[c]["ylocT"]; m["braT"] = resA[c]["braT"]; m["gatesT"] = resA[c]["gatesT"]
            brcT = np.concatenate([resB[b * 4 + hp]["brcT_hp"] for hp in range(4)], axis=0)
            m["brcT"] = np.ascontiguousarray(brcT[:, idx])
            m["x_tok"] = xtoks[c]
            if not last:
                for k, v_ in nextp.items():
                    m["n_" + k] = v_
            maps.append(m)
        return maps

    pA = host_layer_params(inp, 0)
    mapsA = []
    for c in cores:
        m = dict(consts); m.update(pA); m["xT"] = np.ascontiguousarray(x_tok[c].T)
        mapsA.append(m)
    resA = run_bass_kernel_spmd(_prog("A", build_A), mapsA, core_ids=cores).results
    resB = run_bass_kernel_spmd(_prog("B", build_B), exchange_for_B(resA), core_ids=cores).results
    res3 = run_bass_kernel_spmd(_prog("C0", lambda: build_C(False)), maps_for_C(0, resA, resB, x_tok, False), core_ids=cores).results
    resA1 = [{k: r_["n_" + k] for k in A_OUT_SHAPES} for r_ in res3]
    x1_tok = [np.asarray(r_["out_tok"]) for r_ in res3]
    resB1 = run_bass_kernel_spmd(_prog("B", build_B), exchange_for_B(resA1), core_ids=cores).results
    res5 = run_bass_kernel_spmd(_prog("C1", lambda: build_C(True)), maps_for_C(1, resA1, resB1, x1_tok, True), core_ids=cores).results
    out = np.zeros((2, 16384, 1024), np.float32)
    for c in cores:
        b, idx = tok[c]
        out[b, idx] = np.asarray(res5[c]["out_tok"])
    return out
```
